# Optimizing a Trainium2 kernel written in Bass

```python
import math
import jax, jax.numpy as jnp
from jax import lax
import numpy as np

D_MODEL = 4096
BATCH = 2
SEQ = 8192
DEPTH = 2

N_MIXERS = 2
N_DIFF_LAYERS = (DEPTH + 1) // 2
N_MOBA_LAYERS = DEPTH // 2

ROPE_THETA = 500000.0

DIFF_HEADS = D_MODEL // 256
DIFF_QK_DIM = 128
DIFF_V_DIM = 2 * DIFF_QK_DIM
DIFF_Q_COLS = DIFF_HEADS * 2 * DIFF_QK_DIM
DIFF_V_COLS = DIFF_HEADS * DIFF_V_DIM
DENSE_Q_BLOCK = 128

MOBA_HEADS = D_MODEL // 128
MOBA_HEAD_DIM = 128
MOBA_COLS = MOBA_HEADS * MOBA_HEAD_DIM
MOBA_BLOCK = 256
MOBA_TOPK = 3
MOBA_Q_CHUNK = 16

ROT_DIM = DIFF_QK_DIM // 4

N_EXPERTS = 32
TOP_K = 4
D_EXPERT = 768
SWIGLU_LIMIT = 7.0
SWIGLU_ALPHA = 1.702
MOE_ROW_BLOCK = 128

LN_EPS = 1e-5
RMS_EPS = 1e-5
DEEPNORM_ALPHA = (2 * DEPTH) ** 0.25
DEEPNORM_BETA = (8 * DEPTH) ** -0.25

kernel_name = 'hybrid_diffattn_moba_moe_deepnorm'


def layer_norm(x, g, b):
    xf = x.astype(jnp.float32)
    mu = jnp.mean(xf, axis=-1, keepdims=True)
    var = jnp.mean(jnp.square(xf - mu), axis=-1, keepdims=True)
    y = (xf - mu) * lax.rsqrt(var + LN_EPS) * g.astype(jnp.float32) + b.astype(jnp.float32)
    return y.astype(x.dtype)


def rope_tables(positions):
    inv = 1.0 / (ROPE_THETA ** (jnp.arange(0, ROT_DIM, 2, dtype=jnp.float32) / ROT_DIM))
    ang = positions.astype(jnp.float32)[..., None] * inv
    return jnp.cos(ang), jnp.sin(ang)


def apply_partial_rope(x, cos, sin):
    half = ROT_DIM // 2
    c = cos.astype(x.dtype)
    s = sin.astype(x.dtype)
    x1 = x[..., :half]
    x2 = x[..., half:ROT_DIM]
    return jnp.concatenate([x1 * c - x2 * s, x2 * c + x1 * s, x[..., ROT_DIM:]], axis=-1)


def diff_attention(x, cos, sin, w_qkv, lq1, lk1, lq2, lk2, subln_g, w_o, lambda_init):
    B, S, _ = x.shape
    H, d = DIFF_HEADS, DIFF_QK_DIM
    qkv = x @ w_qkv
    q = qkv[..., :DIFF_Q_COLS].reshape(B, S, H, 2, d).transpose(0, 2, 3, 1, 4)
    k = qkv[..., DIFF_Q_COLS:2 * DIFF_Q_COLS].reshape(B, S, H, 2, d).transpose(0, 2, 3, 1, 4)
    v = qkv[..., 2 * DIFF_Q_COLS:].reshape(B, S, H, DIFF_V_DIM).transpose(0, 2, 1, 3)
    c5, s5 = cos[:, None, None], sin[:, None, None]
    q = apply_partial_rope(q, c5, s5)
    k = apply_partial_rope(k, c5, s5)
    lam = (jnp.exp(jnp.sum(lq1.astype(jnp.float32) * lk1.astype(jnp.float32)))
           - jnp.exp(jnp.sum(lq2.astype(jnp.float32) * lk2.astype(jnp.float32)))
           + lambda_init)
    scale = d ** -0.5
    kpos = jnp.arange(S)

    def q_block(i):
        qs = lax.dynamic_slice_in_dim(q, i * DENSE_Q_BLOCK, DENSE_Q_BLOCK, axis=3)
        sc = jnp.einsum('bhcqd,bhckd->bhcqk', qs, k).astype(jnp.float32) * scale
        qpos = i * DENSE_Q_BLOCK + jnp.arange(DENSE_Q_BLOCK)
        sc = jnp.where(kpos[None, :] <= qpos[:, None], sc, -jnp.inf)
        p = jax.nn.softmax(sc, axis=-1)
        a = p[:, :, 0] - lam * p[:, :, 1]
        return jnp.einsum('bhqk,bhkv->bhqv', a.astype(v.dtype), v)

    o = lax.map(q_block, jnp.arange(S // DENSE_Q_BLOCK))
    o = o.transpose(1, 2, 0, 3, 4).reshape(B, H, S, DIFF_V_DIM)
    of = o.astype(jnp.float32)
    of = of * lax.rsqrt(jnp.mean(jnp.square(of), axis=-1, keepdims=True) + RMS_EPS)
    of = of * subln_g.astype(jnp.float32) * (1.0 - lambda_init)
    o = of.astype(x.dtype).transpose(0, 2, 1, 3).reshape(B, S, DIFF_V_COLS)
    return o @ w_o


def moba_attention(x, cos, sin, w_qkv, w_o):
    B, S, _ = x.shape
    H, d, BLK, C = MOBA_HEADS, MOBA_HEAD_DIM, MOBA_BLOCK, MOBA_Q_CHUNK
    qkv = x @ w_qkv
    q = qkv[..., :MOBA_COLS].reshape(B, S, H, d).transpose(0, 2, 1, 3)
    k = qkv[..., MOBA_COLS:2 * MOBA_COLS].reshape(B, S, H, d).transpose(0, 2, 1, 3)
    v = qkv[..., 2 * MOBA_COLS:].reshape(B, S, H, d).transpose(0, 2, 1, 3)
    c4, s4 = cos[:, None], sin[:, None]
    q = apply_partial_rope(q, c4, s4)
    k = apply_partial_rope(k, c4, s4)
    pad = (-S) % BLK
    kb = jnp.pad(k, ((0, 0), (0, 0), (0, pad), (0, 0)))
    vb = jnp.pad(v, ((0, 0), (0, 0), (0, pad), (0, 0)))
    NB = (S + pad) // BLK
    kb = kb.reshape(B, H, NB, BLK, d)
    vb = vb.reshape(B, H, NB, BLK, d)
    kmean = jnp.mean(kb.astype(jnp.float32), axis=3)
    n_sel = min(MOBA_TOPK, NB)
    scale = d ** -0.5
    blk_ids = jnp.arange(NB)
    b_idx = jnp.arange(B)[:, None, None, None]
    h_idx = jnp.arange(H)[None, :, None, None]

    def q_chunk(c):
        start = c * C
        qc = lax.dynamic_slice_in_dim(q, start, C, axis=2)
        qpos = start + jnp.arange(C)
        own = start // BLK
        gate = jnp.einsum('bhqd,bhnd->bhqn', qc.astype(jnp.float32), kmean)
        gate = jnp.where(blk_ids < own, gate, -jnp.inf)
        _, sel = lax.top_k(gate, n_sel)
        sel_valid = jnp.arange(n_sel) < own
        ksel = kb[b_idx, h_idx, sel]
        vsel = vb[b_idx, h_idx, sel]
        s_sel = jnp.einsum('bhqd,bhqnkd->bhqnk', qc, ksel).astype(jnp.float32) * scale
        s_sel = jnp.where(sel_valid[:, None], s_sel, -jnp.inf).reshape(B, H, C, n_sel * BLK)
        k_own = lax.dynamic_index_in_dim(kb, own, axis=2, keepdims=False)
        v_own = lax.dynamic_index_in_dim(vb, own, axis=2, keepdims=False)
        s_own = jnp.einsum('bhqd,bhkd->bhqk', qc, k_own).astype(jnp.float32) * scale
        kpos_own = own * BLK + jnp.arange(BLK)
        s_own = jnp.where(kpos_own[None, :] <= qpos[:, None], s_own, -jnp.inf)
        p = jax.nn.softmax(jnp.concatenate([s_sel, s_own], axis=-1), axis=-1).astype(v.dtype)
        p_sel = p[..., :n_sel * BLK].reshape(B, H, C, n_sel, BLK)
        p_own = p[..., n_sel * BLK:]
        return (jnp.einsum('bhqnk,bhqnkd->bhqd', p_sel, vsel)
                + jnp.einsum('bhqk,bhkd->bhqd', p_own, v_own))

    o = lax.map(q_chunk, jnp.arange(S // C))
    o = o.transpose(1, 0, 3, 2, 4).reshape(B, S, MOBA_COLS)
    return o @ w_o


def moe_ffn(x, router_w, router_b, w_gate, b_gate, w_up, b_up, w_down, b_down):
    B, S, D = x.shape
    G = MOE_ROW_BLOCK
    xf = x.reshape(B * S, D)
    N = xf.shape[0]
    logits = (xf @ router_w + router_b).astype(jnp.float32)
    top_val, top_idx = lax.top_k(logits, TOP_K)
    top_w = jax.nn.softmax(top_val, axis=-1)
    NK = N * TOP_K
    e_flat = top_idx.reshape(NK)
    w_flat = top_w.reshape(NK)
    tok_flat = jnp.arange(NK, dtype=jnp.int32) // TOP_K
    counts = jnp.bincount(e_flat, length=N_EXPERTS)
    padded = (counts + G - 1) // G * G
    pend = jnp.cumsum(padded)
    pstart = pend - padded
    ustart = jnp.cumsum(counts) - counts
    order = jnp.argsort(e_flat)
    e_sorted = e_flat[order]
    dest = pstart[e_sorted] + jnp.arange(NK) - ustart[e_sorted]
    n_blocks = -(-NK // G) + N_EXPERTS
    P = n_blocks * G
    row_tok = jnp.zeros((P,), jnp.int32).at[dest].set(tok_flat[order])
    row_w = jnp.zeros((P,), jnp.float32).at[dest].set(w_flat[order])
    block_exp = jnp.minimum(
        jnp.searchsorted(pend, jnp.arange(n_blocks) * G, side='right'), N_EXPERTS - 1)

    def expert_block(args):
        toks, wts, e = args
        xb = xf[toks]
        g = jnp.minimum(xb @ w_gate[e] + b_gate[e], SWIGLU_LIMIT)
        u = jnp.clip(xb @ w_up[e] + b_up[e], -SWIGLU_LIMIT, SWIGLU_LIMIT)
        h = g * jax.nn.sigmoid(SWIGLU_ALPHA * g) * (u + 1.0)
        y = h @ w_down[e] + b_down[e]
        return y * wts[:, None].astype(y.dtype)

    ys = lax.map(expert_block, (row_tok.reshape(n_blocks, G), row_w.reshape(n_blocks, G), block_exp))
    out = jax.ops.segment_sum(ys.reshape(P, D), row_tok, num_segments=N)
    return out.reshape(B, S, D)


def setup_inputs(seed: int = 0) -> dict:
    key = jax.random.key(seed)
    ks = jax.random.split(key, 24)
    D, E, F = D_MODEL, N_EXPERTS, D_EXPERT

    def nrm(k, shape, scale):
        return jax.random.normal(k, shape, jnp.float32) * scale

    x = nrm(ks[0], (BATCH, SEQ, D), 1.0)
    offsets = jax.random.randint(ks[1], (BATCH, 1), 0, 4096, dtype=jnp.int32)
    positions = offsets + jnp.arange(SEQ, dtype=jnp.int32)[None, :]
    return {
        'x': x,
        'positions': positions,
        'diff_w_qkv': nrm(ks[2], (N_DIFF_LAYERS, D, 2 * DIFF_Q_COLS + DIFF_V_COLS), D ** -0.5),
        'diff_lambda_q1': nrm(ks[3], (N_DIFF_LAYERS, DIFF_QK_DIM), 0.1),
        'diff_lambda_k1': nrm(ks[4], (N_DIFF_LAYERS, DIFF_QK_DIM), 0.1),
        'diff_lambda_q2': nrm(ks[5], (N_DIFF_LAYERS, DIFF_QK_DIM), 0.1),
        'diff_lambda_k2': nrm(ks[6], (N_DIFF_LAYERS, DIFF_QK_DIM), 0.1),
        'diff_subln_g': 1.0 + nrm(ks[7], (N_DIFF_LAYERS, DIFF_V_DIM), 0.02),
        'diff_w_o': nrm(ks[8], (N_DIFF_LAYERS, DIFF_V_COLS, D), DIFF_V_COLS ** -0.5 * DEEPNORM_BETA),
        'moba_w_qkv': nrm(ks[9], (N_MOBA_LAYERS, D, 3 * MOBA_COLS), D ** -0.5),
        'moba_w_o': nrm(ks[10], (N_MOBA_LAYERS, MOBA_COLS, D), MOBA_COLS ** -0.5 * DEEPNORM_BETA),
        'ln_mix_g': 1.0 + nrm(ks[11], (DEPTH, D), 0.02),
        'ln_mix_b': nrm(ks[12], (DEPTH, D), 0.02),
        'router_w': nrm(ks[13], (DEPTH, D, E), D ** -0.5),
        'router_b': nrm(ks[14], (DEPTH, E), 0.01),
        'exp_w_gate': nrm(ks[15], (DEPTH, E, D, F), D ** -0.5),
        'exp_b_gate': nrm(ks[16], (DEPTH, E, F), 0.01),
        'exp_w_up': nrm(ks[17], (DEPTH, E, D, F), D ** -0.5),
        'exp_b_up': nrm(ks[18], (DEPTH, E, F), 0.01),
        'exp_w_down': nrm(ks[19], (DEPTH, E, F, D), F ** -0.5 * DEEPNORM_BETA),
        'exp_b_down': nrm(ks[20], (DEPTH, E, D), 0.01),
        'ln_ffn_g': 1.0 + nrm(ks[21], (DEPTH, D), 0.02),
        'ln_ffn_b': nrm(ks[22], (DEPTH, D), 0.02),
    }


def reference(x, positions, diff_w_qkv, diff_lambda_q1, diff_lambda_k1, diff_lambda_q2,
              diff_lambda_k2, diff_subln_g, diff_w_o, moba_w_qkv, moba_w_o, ln_mix_g, ln_mix_b,
              router_w, router_b, exp_w_gate, exp_b_gate, exp_w_up, exp_b_up, exp_w_down,
              exp_b_down, ln_ffn_g, ln_ffn_b):
    cos, sin = rope_tables(positions)
    for i in range(DEPTH):
        j = i // N_MIXERS
        if i % N_MIXERS == 0:
            lambda_init = 0.8 - 0.6 * math.exp(-0.3 * i)
            mix = diff_attention(x, cos, sin, diff_w_qkv[j], diff_lambda_q1[j], diff_lambda_k1[j],
                                 diff_lambda_q2[j], diff_lambda_k2[j], diff_subln_g[j],
                                 diff_w_o[j], lambda_init)
        else:
            mix = moba_attention(x, cos, sin, moba_w_qkv[j], moba_w_o[j])
        x = layer_norm(DEEPNORM_ALPHA * x + mix, ln_mix_g[i], ln_mix_b[i])
        ffn = moe_ffn(x, router_w[i], router_b[i], exp_w_gate[i], exp_b_gate[i], exp_w_up[i],
                      exp_b_up[i], exp_w_down[i], exp_b_down[i])
        x = layer_norm(DEEPNORM_ALPHA * x + ffn, ln_ffn_g[i], ln_ffn_b[i])
    return x
```

```python
import math
import numpy as np
import concourse.bass as bass
import concourse.mybir as mybir
from concourse.bass_utils import run_bass_kernel_spmd

F32 = mybir.dt.float32
BF16 = mybir.dt.bfloat16
I32 = mybir.dt.int32
ALU = mybir.AluOpType
AF = mybir.ActivationFunctionType
AX = mybir.AxisListType
SEM_ROT = 100000
NEG = -30000.0


class Res:
    __slots__ = ("w", "r")

    def __init__(self):
        self.w = None
        self.r = []


class Sched:
    def __init__(self, nc, n_dma_sems=16):
        self.nc = nc
        self.lists = {e: [] for e in ("pe", "dve", "act", "pool", "sp")}
        self.cur_sem = {}
        self.cur_cnt = {}
        for e in ("pe", "dve", "act", "pool"):
            self._new_sem(e)
        self.seen = {e: {} for e in self.lists}
        self.dma_sems = {}
        self.dma_idx = {}
        for q in ("sp", "poolq"):
            self.dma_sems[q] = [[nc.alloc_semaphore(name=f"d_{q}_{i}"), 0] for i in range(n_dma_sems)]
            self.dma_idx[q] = 0
        self.n_ops = 0

    def _new_sem(self, e):
        self.cur_sem[e] = self.nc.alloc_semaphore(name=f"s_{e}_{len(self.lists[e])}")
        self.cur_cnt[e] = 0

    @staticmethod
    def _deps(reads, writes):
        deps = {}

        def add(tok):
            if tok is None:
                return
            s, v = tok
            k = id(s)
            if k not in deps or deps[k][1] < v:
                deps[k] = (s, v)
        for r in reads:
            add(r.w)
        for w in writes:
            add(w.w)
            for t in w.r:
                add(t)
        return deps

    def _finish(self, st, deps, fn, tok, inc, reads, writes, skip_sem=None):
        waits = []
        for k, (s, v) in deps.items():
            if skip_sem is not None and s is skip_sem:
                continue
            if self.seen[st].get(k, 0) >= v:
                continue
            self.seen[st][k] = v
            waits.append((s, v))
        self.lists[st].append((waits, fn, tok[0], inc))
        for r in reads:
            r.r.append(tok)
            if len(r.r) > 64:
                r.r = r.r[-64:] if False else r.r
        for w in writes:
            w.w = tok
            w.r = []
        self.n_ops += 1
        return tok

    def op(self, eng, name, reads=(), writes=(), **kw):
        deps = self._deps(reads, writes)
        if self.cur_cnt[eng] >= SEM_ROT:
            self._new_sem(eng)
        self.cur_cnt[eng] += 1
        tok = (self.cur_sem[eng], self.cur_cnt[eng])
        fn = (lambda e, name=name, kw=kw: getattr(e, name)(**kw))
        return self._finish(eng, deps, fn, tok, 1, reads, writes,
                            skip_sem=self.cur_sem["pe"] if eng == "pe" else None)

    def dma(self, q, reads=(), writes=(), **kw):
        st = "pool" if q == "poolq" else "sp"
        deps = self._deps(reads, writes)
        pool = self.dma_sems[q]
        i = self.dma_idx[q]
        self.dma_idx[q] = (i + 1) % len(pool)
        ent = pool[i]
        if ent[1] > 0:
            k = id(ent[0])
            if k not in deps or deps[k][1] < ent[1]:
                deps[k] = (ent[0], ent[1])
        ent[1] += 16
        tok = (ent[0], ent[1])
        fn = (lambda e, kw=kw: e.dma_start(**kw))
        return self._finish(st, deps, fn, tok, 16, reads, writes)

    def dma_fn(self, q, name, reads=(), writes=(), **kw):
        st = "pool" if q == "poolq" else "sp"
        deps = self._deps(reads, writes)
        pool = self.dma_sems[q]
        i = self.dma_idx[q]
        self.dma_idx[q] = (i + 1) % len(pool)
        ent = pool[i]
        if ent[1] > 0:
            k = id(ent[0])
            if k not in deps or deps[k][1] < ent[1]:
                deps[k] = (ent[0], ent[1])
        ent[1] += 16
        tok = (ent[0], ent[1])
        fn = (lambda e, name=name, kw=kw: getattr(e, name)(**kw))
        return self._finish(st, deps, fn, tok, 16, reads, writes)

    def barrier(self):
        toks = [(self.cur_sem[e], self.cur_cnt[e]) for e in ("pe", "dve", "act", "pool") if self.cur_cnt[e] > 0]
        toks += [(ent[0], ent[1]) for q in self.dma_sems for ent in self.dma_sems[q] if ent[1] > 0]
        for st in self.lists:
            waits = []
            for (s_, v) in toks:
                if self.seen[st].get(id(s_), 0) >= v:
                    continue
                self.seen[st][id(s_)] = v
                waits.append((s_, v))
            self.lists[st].append((waits, None, None, 0))

    def wait_all(self, eng, toks):
        self.lists[eng].append(([(s, v) for (s, v) in toks], None, None, 0))

    def emit(self):
        nc = self.nc
        lists = self.lists
        with nc.Block() as block:
            def run(engine, items):
                for waits, fn, sem, inc in items:
                    for (s, v) in waits:
                        engine.wait_ge(s, v)
                    if fn is not None:
                        fn(engine).then_inc(sem, inc)

            @block.tensor
            def _(e):
                run(e, lists["pe"])

            @block.vector
            def _(e):
                run(e, lists["dve"])

            @block.scalar
            def _(e):
                run(e, lists["act"])

            @block.gpsimd
            def _(e):
                run(e, lists["pool"])

            @block.sync
            def _(e):
                run(e, lists["sp"])


class Buf:
    def __init__(self, t):
        self.t = t
        self.r = Res()


def build_program(cfg):
    D, S, E, F, depth = cfg["D"], cfg["S"], cfg["E"], cfg["F"], cfg["depth"]
    TOPK = cfg["topk"]
    theta = cfg["theta"]
    DC = D // 128
    NT = S // 128
    NG = S // 512
    NM = D // 128
    FC = F // 128
    NB = S // 256
    NBP = max(NB, 8)
    alpha = float((2 * depth) ** 0.25)
    n_diff = (depth + 1) // 2
    n_moba = depth // 2
    scale = float(128 ** -0.5)

    nc = bass.Bass("TRN2", target_bir_lowering=False)
    ext = {}

    def din(name, shape, dt=F32):
        ext[name] = nc.dram_tensor(name, list(shape), dt, kind="ExternalInput").ap()
        return ext[name]
    x_in = din("x", [S, D])
    pos_in = din("positions", [S, 1], I32)
    diff_wqkv = din("diff_w_qkv", [n_diff, D, 3 * D])
    lq1 = din("diff_lambda_q1", [n_diff, 128]); lk1 = din("diff_lambda_k1", [n_diff, 128])
    lq2 = din("diff_lambda_q2", [n_diff, 128]); lk2 = din("diff_lambda_k2", [n_diff, 128])
    subg = din("diff_subln_g", [n_diff, 256])
    diff_wo = din("diff_w_o", [n_diff, D, D])
    moba_wqkv = din("moba_w_qkv", [max(n_moba, 1), D, 3 * D])
    moba_wo = din("moba_w_o", [max(n_moba, 1), D, D])
    ln_mix_g = din("ln_mix_g", [depth, D]); ln_mix_b = din("ln_mix_b", [depth, D])
    router_w = din("router_w", [depth, D, E]); router_b = din("router_b", [depth, E])
    wg_in = din("exp_w_gate", [depth, E, D, F]); bg_in = din("exp_b_gate", [depth, E, F])
    wu_in = din("exp_w_up", [depth, E, D, F]); bu_in = din("exp_b_up", [depth, E, F])
    wd_in = din("exp_w_down", [depth, E, F, D]); bd_in = din("exp_b_down", [depth, E, D])
    ln_ffn_g = din("ln_ffn_g", [depth, D]); ln_ffn_b = din("ln_ffn_b", [depth, D])
    y_out = nc.dram_tensor("y", [S, D], F32, kind="ExternalOutput").ap()

    def dscr(name, shape, dt):
        return nc.dram_tensor(name, list(shape), dt, kind="Internal").ap()
    QT = dscr("QT", [NM, 128, S], BF16); r_QT = Res()
    KT = dscr("KT", [NM, 128, S], BF16); r_KT = Res()
    Vd = dscr("Vd", [S, D], BF16); r_V = Res()
    AO = dscr("AO", [S, D], F32); r_AO = Res()
    X1 = dscr("X1", [S, D], F32); r_X1 = Res()
    X2 = dscr("X2", [S, D], F32); r_X2 = Res()
    r_in = Res()
    r_y = Res()
    _NBLK = (S * TOPK) // 128 + E
    CW = min(1024, D)
    NCH = D // CW
    XS = [dscr(f"XS{i}", [_NBLK * 128, CW], F32) for i in range(NCH)]; r_XS = Res()
    YS = [dscr(f"YS{i}", [_NBLK * 128, CW], F32) for i in range(NCH)]; r_YS = Res()
    WG2 = dscr("WG2", [E * (F // 128) * 128, D], BF16)
    WU2 = dscr("WU2", [E * (F // 128) * 128, D], BF16)
    WD2 = dscr("WD2", [E * F, D], BF16)
    BG2 = dscr("BG2", [E * 128, F // 128], F32)
    BU2 = dscr("BU2", [E * 128, F // 128], F32)
    r_W2 = Res()

    Sc = Sched(nc)
    op, dma = Sc.op, Sc.dma

    uid = [0]

    def sb(name, shape, dt, es=None):
        if es is None:
            return Buf(nc.alloc_sbuf_tensor(name, list(shape), dt))
        uid[0] += 1
        return Buf(es.enter_context(nc.sbuf_tensor(f"{name}_{uid[0]}", list(shape), dt)))

    def bc(ap2d):
        return ap2d.broadcast_to([128, ap2d.shape[1]])
    P = [Buf(nc.alloc_psum_tensor(f"P{i}", [128, 512], F32)) for i in range(7)]
    PB = Buf(nc.alloc_psum_tensor("PB", [128, 1024], BF16))

    from contextlib import ExitStack
    identf = sb("identf", [128, 128], F32)
    identb = sb("identb", [128, 128], BF16)
    eoh = sb("eoh", [32, NBP * 128], BF16)
    cmask = [sb(f"cmask{i}", [128, 512], F32) for i in range(4)]
    sinT = sb("sinT", [128, NT * 16], F32)
    cosT = sb("cosT", [128, NT * 16], F32)
    m8 = sb("m8", [128, 8], F32)
    stats = sb("stats", [128, max(1, D // 512) * 6], F32)
    mv = sb("mv", [128, 2], F32)
    rstd = sb("rstd", [128, 1], F32)
    with ExitStack() as es:
        io = sb("io", [128, 128], I32, es)
        op("pool", "iota", writes=[io.r], out=io.t[:], pattern=[[1, 128]], base=0, channel_multiplier=-1)
        op("dve", "tensor_scalar", reads=[io.r], writes=[identf.r], out=identf.t[:], in0=io.t[:], scalar1=0, scalar2=None, op0=ALU.is_equal)
        op("dve", "tensor_copy", reads=[identf.r], writes=[identb.r], out=identb.t[:], in_=identf.t[:])
        io2 = sb("io2", [32, NBP * 128], I32, es)
        op("pool", "iota", writes=[io2.r], out=io2.t[:], pattern=[[1, NBP], [0, 128]], base=0, channel_multiplier=-1)
        op("dve", "tensor_scalar", reads=[io2.r], writes=[eoh.r], out=eoh.t[:], in0=io2.t[:], scalar1=0, scalar2=None, op0=ALU.is_equal)
        iom = sb("iom", [128, 512], I32, es)
        op("pool", "iota", writes=[iom.r], out=iom.t[:], pattern=[[1, 512]], base=0, channel_multiplier=-1)
        for i in range(4):
            op("dve", "tensor_scalar", reads=[iom.r], writes=[cmask[i].r], out=cmask[i].t[:], in0=iom.t[:], scalar1=128 * i, scalar2=None, op0=ALU.is_ge)
            op("dve", "tensor_scalar", reads=[cmask[i].r], writes=[cmask[i].r], out=cmask[i].t[:], in0=cmask[i].t[:], scalar1=-1.0, scalar2=-NEG, op0=ALU.add, op1=ALU.mult)
        posf = sb("posf", [128, NT], F32, es)
        ang = sb("ang", [128, NT * 16], F32, es)
        ki = sb("ki", [128, NT * 16], I32, es)
        kf = sb("kf", [128, NT * 16], F32, es)
        posr = sb("posr", [NT, 128], I32, es)
        posrf = sb("posrf", [NT, 128], F32, es)
        dma("sp", reads=[r_in], writes=[posr.r], out=posr.t[:], in_=pos_in.rearrange("(t p) o -> t (p o)", p=128))
        op("dve", "tensor_copy", reads=[posr.r], writes=[posrf.r], out=posrf.t[:], in_=posr.t[:])
        op("pe", "transpose", reads=[posrf.r, identf.r], writes=[P[0].r], out=P[0].t[:, 0:NT], in_=posrf.t[:], identity=identf.t[0:NT, 0:NT])
        op("act", "activation", reads=[P[0].r], writes=[posf.r], out=posf.t[:], in_=P[0].t[:, 0:NT], func=AF.Copy)
        inv = (np.float32(1.0) / (np.float32(theta) ** (np.arange(0, 32, 2, dtype=np.float32) / np.float32(32)))).astype(np.float32)
        ang3 = ang.t[:].rearrange("p (t i) -> p t i", i=16)
        for i in range(16):
            op("dve", "tensor_scalar", reads=[posf.r], writes=[ang.r], out=ang3[:, :, i], in0=posf.t[:], scalar1=float(inv[i]), scalar2=None, op0=ALU.mult)
        C1 = 6.28125
        C2 = float(2 * np.pi - 6.28125)
        PI = float(np.pi)

        def wrap(dst, src):
            op("dve", "tensor_scalar", reads=[src.r], writes=[kf.r], out=kf.t[:], in0=src.t[:], scalar1=PI, scalar2=-2 * PI, op0=ALU.is_gt, op1=ALU.mult)
            op("dve", "tensor_tensor", reads=[src.r, kf.r], writes=[dst.r], out=dst.t[:], in0=src.t[:], in1=kf.t[:], op=ALU.add)
            op("dve", "tensor_scalar", reads=[src.r], writes=[kf.r], out=kf.t[:], in0=src.t[:], scalar1=-PI, scalar2=2 * PI, op0=ALU.is_lt, op1=ALU.mult)
            op("dve", "tensor_tensor", reads=[dst.r, kf.r], writes=[dst.r], out=dst.t[:], in0=dst.t[:], in1=kf.t[:], op=ALU.add)
            op("dve", "tensor_scalar", reads=[dst.r], writes=[dst.r], out=dst.t[:], in0=dst.t[:], scalar1=PI, scalar2=-PI, op0=ALU.min, op1=ALU.max)
        op("dve", "tensor_scalar", reads=[ang.r], writes=[ki.r], out=ki.t[:], in0=ang.t[:], scalar1=float(1 / (2 * np.pi)), scalar2=None, op0=ALU.mult)
        op("dve", "tensor_copy", reads=[ki.r], writes=[kf.r], out=kf.t[:], in_=ki.t[:])
        op("dve", "scalar_tensor_tensor", reads=[kf.r, ang.r], writes=[ang.r], out=ang.t[:], in0=kf.t[:], scalar=-C1, in1=ang.t[:], op0=ALU.mult, op1=ALU.add)
        op("dve", "scalar_tensor_tensor", reads=[kf.r, ang.r], writes=[ang.r], out=ang.t[:], in0=kf.t[:], scalar=-C2, in1=ang.t[:], op0=ALU.mult, op1=ALU.add)
        wrap(sinT, ang)
        op("dve", "tensor_scalar", reads=[sinT.r], writes=[ang.r], out=ang.t[:], in0=sinT.t[:], scalar1=PI / 2, scalar2=None, op0=ALU.add)
        wrap(cosT, ang)
        op("act", "activation", reads=[sinT.r], writes=[sinT.r], out=sinT.t[:], in_=sinT.t[:], func=AF.Sin)
        op("act", "activation", reads=[cosT.r], writes=[cosT.r], out=cosT.t[:], in_=cosT.t[:], func=AF.Sin)
        Sc.barrier()
    sin3 = sinT.t[:].rearrange("p (t i) -> p t i", i=16)
    cos3 = cosT.t[:].rearrange("p (t i) -> p t i", i=16)
    cnt = {"w": 0, "p": 0, "st": 0, "ts": 0, "xr": 0}

    def load_rows(src_ap, r_src, tile_idx, buf):
        dma("sp", reads=[r_src], writes=[buf.r], out=buf.t[:], in_=src_ap[tile_idx * 128:(tile_idx + 1) * 128, :])

    def layer_norm_rows(hb, gbc, bbc, out_ap, r_out, tile_idx):
        nch = max(1, D // 512)
        w = D // nch
        for c in range(nch):
            op("dve", "bn_stats", reads=[hb.r], writes=[stats.r], out=stats.t[:, c * 6:(c + 1) * 6], in_=hb.t[:, c * w:(c + 1) * w])
        op("dve", "bn_aggr", reads=[stats.r], writes=[mv.r], out=mv.t[:], in_=stats.t[:, 0:nch * 6])
        op("dve", "tensor_scalar", reads=[mv.r], writes=[rstd.r], out=rstd.t[:], in0=mv.t[:, 1:2], scalar1=float(cfg["ln_eps"]), scalar2=None, op0=ALU.add)
        op("act", "activation", reads=[rstd.r], writes=[rstd.r], out=rstd.t[:], in_=rstd.t[:], func=AF.Sqrt)
        op("dve", "reciprocal", reads=[rstd.r], writes=[rstd.r], out=rstd.t[:], in_=rstd.t[:])
        op("dve", "tensor_scalar", reads=[hb.r, mv.r, rstd.r], writes=[hb.r], out=hb.t[:], in0=hb.t[:], scalar1=mv.t[:, 0:1], scalar2=rstd.t[:, 0:1], op0=ALU.subtract, op1=ALU.mult)
        op("dve", "tensor_tensor", reads=[hb.r, gbc.r], writes=[hb.r], out=hb.t[:], in0=hb.t[:], in1=gbc.t[:], op=ALU.mult)
        op("dve", "tensor_tensor", reads=[hb.r, bbc.r], writes=[hb.r], out=hb.t[:], in0=hb.t[:], in1=bbc.t[:], op=ALU.add)
        dma("sp", reads=[hb.r], writes=[r_out], out=out_ap[tile_idx * 128:(tile_idx + 1) * 128, :], in_=hb.t[:])

    QKB = 3 * D // 512
    class _Stop(Exception):
        pass
    stop = cfg.get('stop')
    try:
      if stop == 'setup':
          raise _Stop()
      for L in range(depth):
          is_diff = (L % 2 == 0)
          j = L // 2
          wqkv = (diff_wqkv if is_diff else moba_wqkv)[j]
          wo = (diff_wo if is_diff else moba_wo)[j]
          x_src, r_src = (x_in, r_in) if L == 0 else (X2, r_X2)
          lambda_init = 0.8 - 0.6 * math.exp(-0.3 * L)
          dv = 256 if is_diff else 128

          with ExitStack() as es:
              xrow = [sb("xrow", [128, D], F32, es) for i in range(2)]
              actT = sb("actT", [128, DC, 512], BF16, es)
              wblk = [sb("wblk", [128, DC, 512], BF16, es) for i in range(2)]
              stage = [sb("stage", [128, 512], F32, es) for i in range(2)]
              vstage = [sb("vstage", [128, 512], BF16, es) for i in range(2)]
              tstage = [sb("tstage", [128, 512], BF16, es) for i in range(2)]
              t1 = sb("t1", [128, 64], F32, es)
              for g in range(NG):
                  for s4 in range(4):
                      xb = xrow[cnt["xr"] % 2]
                      cnt["xr"] += 1
                      load_rows(x_src, r_src, g * 4 + s4, xb)
                      for c4 in range(0, DC, 4):
                          pb = P[cnt["p"] % 2]
                          cnt["p"] += 1
                          n = min(4, DC - c4)
                          for jj in range(n):
                              op("pe", "transpose", reads=[xb.r, identf.r], writes=[pb.r], out=pb.t[:, jj * 128:(jj + 1) * 128], in_=xb.t[:, (c4 + jj) * 128:(c4 + jj + 1) * 128], identity=identf.t[:])
                          src = pb.t[:, 0:n * 128].rearrange("p (c t) -> p c t", t=128)
                          if (c4 // 4) % 2 == 0:
                              op("act", "activation", reads=[pb.r], writes=[actT.r], out=actT.t[:, c4:c4 + n, s4 * 128:(s4 + 1) * 128], in_=src, func=AF.Copy)
                          else:
                              op("dve", "tensor_copy", reads=[pb.r], writes=[actT.r], out=actT.t[:, c4:c4 + n, s4 * 128:(s4 + 1) * 128], in_=src)
                  for cb in range(QKB):
                      wb = wblk[cnt["w"] % 2]
                      cnt["w"] += 1
                      dma("poolq", reads=[r_in], writes=[wb.r], out=wb.t[:], in_=wqkv[:, cb * 512:(cb + 1) * 512].rearrange("(c p) n -> p c n", p=128))
                      for s4 in range(4):
                          tt = g * 4 + s4
                          pa = P[2 + cnt["p"] % 2]
                          cnt["p"] += 1
                          for c in range(DC):
                              op("pe", "matmul", reads=[actT.r, wb.r], writes=[pa.r], out=pa.t[:], lhsT=actT.t[:, c, s4 * 128:(s4 + 1) * 128], rhs=wb.t[:, c, :], start=(c == 0), stop=(c == DC - 1))
                          st = stage[cnt["st"] % 2]
                          cnt["st"] += 1
                          op("act", "activation", reads=[pa.r], writes=[st.r], out=st.t[:], in_=pa.t[:], func=AF.Copy)
                          if cfg.get('noqk') and cb * 512 < 2 * D:
                              continue
                          if cb * 512 < 2 * D:
                              for m in range(0 if not cfg.get('norope') else 4, 4):
                                  a = st.t[:, m * 128:m * 128 + 16]
                                  b = st.t[:, m * 128 + 16:m * 128 + 32]
                                  op("dve", "tensor_tensor", reads=[st.r, cosT.r], writes=[t1.r], out=t1.t[:, 0:16], in0=a, in1=cos3[:, tt, :], op=ALU.mult)
                                  op("dve", "tensor_tensor", reads=[st.r, sinT.r], writes=[t1.r], out=t1.t[:, 16:32], in0=b, in1=sin3[:, tt, :], op=ALU.mult)
                                  op("dve", "tensor_tensor", reads=[st.r, cosT.r], writes=[t1.r], out=t1.t[:, 32:48], in0=b, in1=cos3[:, tt, :], op=ALU.mult)
                                  op("dve", "tensor_tensor", reads=[st.r, sinT.r], writes=[t1.r], out=t1.t[:, 48:64], in0=a, in1=sin3[:, tt, :], op=ALU.mult)
                                  op("dve", "tensor_tensor", reads=[t1.r, st.r], writes=[st.r], out=st.t[:, m * 128:m * 128 + 16], in0=t1.t[:, 0:16], in1=t1.t[:, 16:32], op=ALU.subtract)
                                  op("dve", "tensor_tensor", reads=[t1.r, st.r], writes=[st.r], out=st.t[:, m * 128 + 16:m * 128 + 32], in0=t1.t[:, 32:48], in1=t1.t[:, 48:64], op=ALU.add)
                              for m in range(4):
                                  op("pe", "transpose", reads=[st.r, identf.r], writes=[P[4].r], out=P[4].t[:, m * 128:(m + 1) * 128], in_=st.t[:, m * 128:(m + 1) * 128], identity=identf.t[:])
                              ts = tstage[cnt["ts"] % 2]
                              cnt["ts"] += 1
                              op("act", "activation", reads=[P[4].r], writes=[ts.r], out=ts.t[:], in_=P[4].t[:], func=AF.Copy)
                              isq = cb * 512 < D
                              dst, r_dst = (QT, r_QT) if isq else (KT, r_KT)
                              m0 = (cb * 512 - (0 if isq else D)) // 128
                              for mm in range(4):
                                  dma("sp", reads=[ts.r], writes=[r_dst], out=dst[m0 + mm, :, tt * 128:(tt + 1) * 128], in_=ts.t[:, mm * 128:(mm + 1) * 128])
                          else:
                              c0 = cb * 512 - 2 * D
                              vs = vstage[cnt["st"] % 2]
                              op("dve", "tensor_copy", reads=[st.r], writes=[vs.r], out=vs.t[:], in_=st.t[:])
                              dma("sp", reads=[vs.r], writes=[r_V], out=Vd[tt * 128:(tt + 1) * 128, c0:c0 + 512], in_=vs.t[:])
              Sc.barrier()
              if stop == 'A' + str(L):
                  raise _Stop()

          with ExitStack() as es:
              ktb = sb("ktb", [128, S], BF16, es)
              vb = sb("vb", [128, NT, 257], BF16, es)
              qtb = [sb("qtb", [128, 512], BF16, es) for i in range(2)]
              ssb = sb("ssb", [128, 512], F32, es)
              ptb = [sb("ptb", [128, 512], BF16, es) for i in range(3)]
              o1 = sb("o1", [128, NT, 256], F32, es)
              ot = sb("ot", [128, 256], F32, es)
              obf = sb("obf", [128, 256], F32, es)
              rc = sb("rc", [128, 1], F32, es)
              sq = sb("sq", [128, 256], F32, es)
              ssum = sb("ssum", [128, 1], F32, es)
              lamt = sb("lamt", [128, 4], F32, es)
              lv = [sb("lv", [128, 128], F32, es) for i in range(4)]
              gsub = sb("gsub", [128, 256], F32, es)
              kmean = sb("kmean", [128, NBP], F32, es)
              kmeanb = sb("kmeanb", [128, NBP], BF16, es)
              gate = sb("gate", [128, NBP], F32, es)
              selb = sb("selb", [128, 32], F32, es)
              selT = sb("selT", [32, 512], BF16, es)
              acnt = {"q": 0, "p": 0, "ps": 0}
              if is_diff:
                  for i, v in enumerate((lq1, lk1, lq2, lk2)):
                      dma("sp", reads=[r_in], writes=[lv[i].r], out=lv[i].t[:], in_=bc(v[j:j + 1, :]))
                  op("dve", "tensor_tensor", reads=[lv[0].r, lv[1].r], writes=[lv[0].r], out=lv[0].t[:], in0=lv[0].t[:], in1=lv[1].t[:], op=ALU.mult)
                  op("dve", "tensor_tensor", reads=[lv[2].r, lv[3].r], writes=[lv[2].r], out=lv[2].t[:], in0=lv[2].t[:], in1=lv[3].t[:], op=ALU.mult)
                  op("dve", "tensor_reduce", reads=[lv[0].r], writes=[lamt.r], out=lamt.t[:, 0:1], in_=lv[0].t[:], axis=AX.X, op=ALU.add)
                  op("dve", "tensor_reduce", reads=[lv[2].r, lamt.r], writes=[lamt.r], out=lamt.t[:, 1:2], in_=lv[2].t[:], axis=AX.X, op=ALU.add)
                  op("act", "activation", reads=[lamt.r], writes=[lamt.r], out=lamt.t[:, 0:2], in_=lamt.t[:, 0:2], func=AF.Exp)
                  op("dve", "tensor_tensor", reads=[lamt.r], writes=[lamt.r], out=lamt.t[:, 2:3], in0=lamt.t[:, 1:2], in1=lamt.t[:, 0:1], op=ALU.subtract)
                  op("dve", "tensor_scalar", reads=[lamt.r], writes=[lamt.r], out=lamt.t[:, 2:3], in0=lamt.t[:, 2:3], scalar1=-float(lambda_init), scalar2=None, op0=ALU.add)
                  dma("sp", reads=[r_in], writes=[gsub.r], out=gsub.t[:], in_=bc(subg[j:j + 1, :]))
                  op("dve", "tensor_scalar", reads=[gsub.r], writes=[gsub.r], out=gsub.t[:], in0=gsub.t[:], scalar1=float(1.0 - lambda_init), scalar2=None, op0=ALU.mult)
              for m in range(NM):
                  h = m // 2 if is_diff else m
                  which = m % 2 if is_diff else 0
                  dma("sp", reads=[r_KT], writes=[ktb.r], out=ktb.t[:], in_=KT[m, :, :])
                  if which == 0:
                      for t0 in range(0, NT, 16):
                        t1_ = min(NT, t0 + 16)
                        dma("sp", reads=[r_V], writes=[vb.r], out=vb.t[:, t0:t1_, 0:dv], in_=Vd[t0 * 128:t1_ * 128, h * dv:(h + 1) * dv].rearrange("(t p) v -> p t v", p=128))
                      op("dve", "memset", reads=[], writes=[vb.r], ap=vb.t[:, :, dv:dv + 1], constant=1.0)
                  if not is_diff:
                      op("dve", "memset", writes=[kmean.r], ap=kmean.t[:], constant=0.0)
                      op("dve", "tensor_reduce", reads=[ktb.r], writes=[kmean.r], out=kmean.t[:, 0:NB], in_=ktb.t[:].rearrange("d (n k) -> d n k", k=256), axis=AX.X, op=ALU.add)
                      op("dve", "tensor_scalar", reads=[kmean.r], writes=[kmeanb.r], out=kmeanb.t[:], in0=kmean.t[:], scalar1=1.0 / 256, scalar2=None, op0=ALU.mult)
                  for qg in range(NG):
                      qt = qtb[acnt["q"] % 2]
                      acnt["q"] += 1
                      dma("sp", reads=[r_QT], writes=[qt.r], out=qt.t[:], in_=QT[m, :, qg * 512:(qg + 1) * 512])
                      if not is_diff:
                          for s4 in range(4):
                              own = (qg * 512 + s4 * 128) // 256
                              op("dve", "memset", writes=[selb.r], ap=selb.t[:], constant=0.0)
                              if own > 3:
                                  pg = P[6]
                                  op("pe", "matmul", reads=[qt.r, kmeanb.r], writes=[pg.r], out=pg.t[:, 0:NBP], lhsT=qt.t[:, s4 * 128:(s4 + 1) * 128], rhs=kmeanb.t[:], start=True, stop=True)
                                  op("dve", "memset", writes=[gate.r], ap=gate.t[:], constant=-1e30)
                                  op("dve", "tensor_copy", reads=[pg.r], writes=[gate.r], out=gate.t[:, 0:own], in_=pg.t[:, 0:own])
                                  op("dve", "max", reads=[gate.r], writes=[m8.r], out=m8.t[:], in_=gate.t[:])
                                  op("dve", "tensor_scalar", reads=[gate.r, m8.r], writes=[selb.r], out=selb.t[:, 0:own], in0=gate.t[:, 0:own], scalar1=m8.t[:, 2:3], scalar2=-1.0, op0=ALU.is_ge, op1=ALU.add)
                                  op("dve", "tensor_scalar", reads=[selb.r], writes=[selb.r], out=selb.t[:, 0:own], in0=selb.t[:, 0:own], scalar1=-NEG, scalar2=None, op0=ALU.mult)
                              pt_ = P[5]
                              op("pe", "transpose", reads=[selb.r, identf.r], writes=[pt_.r], out=pt_.t[0:32, 0:128], in_=selb.t[:, 0:32], identity=identf.t[:])
                              op("act", "activation", reads=[pt_.r], writes=[selT.r], out=selT.t[:, s4 * 128:(s4 + 1) * 128], in_=pt_.t[0:32, 0:128], func=AF.Copy)
                      q0 = qg * 512
                      nkt = (q0 + 512) // 128
                      po = [P[i] for i in range(4)]
                      for kt in range(nkt):
                          k0 = kt * 128
                          ps = P[4 + acnt["ps"] % 2] if is_diff else P[4]
                          acnt["ps"] += 1
                          use_sel = not is_diff
                          op("pe", "matmul", reads=[ktb.r, qt.r], writes=[ps.r], out=ps.t[:], lhsT=ktb.t[:, k0:k0 + 128], rhs=qt.t[:], start=True, stop=not use_sel)
                          if use_sel:
                              n = kt // 2
                              op("pe", "matmul", reads=[eoh.r, selT.r], writes=[ps.r], out=ps.t[:], lhsT=eoh.t[:, n * 128:(n + 1) * 128], rhs=selT.t[:], start=False, stop=True)
                          pt = ptb[acnt["p"] % 3]
                          acnt["p"] += 1
                          if k0 >= q0:
                              cm = cmask[(k0 - q0) // 128]
                              op("dve", "tensor_tensor", reads=[ps.r, cm.r], writes=[ssb.r], out=ssb.t[:], in0=ps.t[:], in1=cm.t[:], op=ALU.add)
                              op("act", "activation", reads=[ssb.r], writes=[pt.r], out=pt.t[:], in_=ssb.t[:], func=AF.Exp, scale=scale)
                          else:
                              op("act", "activation", reads=[ps.r], writes=[pt.r], out=pt.t[:], in_=ps.t[:], func=AF.Exp, scale=scale)
                          for s4 in range(4):
                              qs0 = q0 + s4 * 128
                              if k0 > qs0 + 127:
                                  continue
                              last_kt = (qs0 + 127) // 128
                              op("pe", "matmul", reads=[pt.r, vb.r], writes=[po[s4].r], out=po[s4].t[:, 0:dv + 1], lhsT=pt.t[:, s4 * 128:(s4 + 1) * 128], rhs=vb.t[:, kt, 0:dv + 1], start=(kt == 0), stop=(kt == last_kt))
                      for s4 in range(4):
                          tt = qg * 4 + s4
                          pz = po[s4]
                          op("dve", "reciprocal", reads=[pz.r], writes=[rc.r], out=rc.t[:], in_=pz.t[:, dv:dv + 1])
                          if is_diff and which == 0:
                              op("dve", "tensor_scalar", reads=[pz.r, rc.r], writes=[o1.r], out=o1.t[:, tt, :], in0=pz.t[:, 0:dv], scalar1=rc.t[:, 0:1], scalar2=None, op0=ALU.mult)
                              continue
                          if is_diff:
                              op("dve", "tensor_scalar", reads=[pz.r, rc.r, lamt.r], writes=[ot.r], out=ot.t[:], in0=pz.t[:, 0:dv], scalar1=rc.t[:, 0:1], scalar2=lamt.t[:, 2:3], op0=ALU.mult, op1=ALU.mult)
                              op("dve", "tensor_tensor", reads=[ot.r, o1.r], writes=[ot.r], out=ot.t[:], in0=ot.t[:], in1=o1.t[:, tt, :], op=ALU.add)
                              op("act", "activation", reads=[ot.r], writes=[sq.r, ssum.r], out=sq.t[:], in_=ot.t[:], func=AF.Square, accum_out=ssum.t[:])
                              op("dve", "tensor_scalar", reads=[ssum.r], writes=[ssum.r], out=ssum.t[:], in0=ssum.t[:], scalar1=1.0 / 256, scalar2=float(cfg["rms_eps"]), op0=ALU.mult, op1=ALU.add)
                              op("act", "activation", reads=[ssum.r], writes=[ssum.r], out=ssum.t[:], in_=ssum.t[:], func=AF.Sqrt)
                              op("dve", "reciprocal", reads=[ssum.r], writes=[ssum.r], out=ssum.t[:], in_=ssum.t[:])
                              op("dve", "scalar_tensor_tensor", reads=[ot.r, ssum.r, gsub.r], writes=[obf.r], out=obf.t[:], in0=ot.t[:], scalar=ssum.t[:, 0:1], in1=gsub.t[:], op0=ALU.mult, op1=ALU.mult)
                          else:
                              op("dve", "tensor_scalar", reads=[pz.r, rc.r], writes=[obf.r], out=obf.t[:, 0:dv], in0=pz.t[:, 0:dv], scalar1=rc.t[:, 0:1], scalar2=None, op0=ALU.mult)
                          dma("sp", reads=[obf.r], writes=[r_AO], out=AO[tt * 128:(tt + 1) * 128, h * dv:(h + 1) * dv], in_=obf.t[:, 0:dv])
              Sc.barrier()
              if stop == 'B' + str(L):
                  raise _Stop()

          OB = max(1, D // 512)
          ncol = min(512, D)
          with ExitStack() as es:
              gbc = sb("gbc", [128, D], F32, es)
              bbc = sb("bbc", [128, D], F32, es)
              hrow = [sb("hrow", [128, D], F32, es) for i in range(2)]
              arow = [sb("arow", [128, D], F32, es) for i in range(2)]
              actT = sb("actT", [128, DC, 256], BF16, es)
              wblk = [sb("wblk", [128, DC, 512], BF16, es) for i in range(1)] * 2
              dma("sp", reads=[r_in], writes=[gbc.r], out=gbc.t[:], in_=bc(ln_mix_g[L:L + 1, :]))
              dma("sp", reads=[r_in], writes=[bbc.r], out=bbc.t[:], in_=bc(ln_mix_b[L:L + 1, :]))
              for g2 in range(S // 256):
                  for s2 in range(2):
                      ab = arow[s2]
                      load_rows(AO, r_AO, g2 * 2 + s2, ab)
                      for c4 in range(0, DC, 4):
                          n = min(4, DC - c4)
                          pb = P[4 + (c4 // 4) % 2]
                          for jj in range(n):
                              op("pe", "transpose", reads=[ab.r, identf.r], writes=[pb.r], out=pb.t[:, jj * 128:(jj + 1) * 128], in_=ab.t[:, (c4 + jj) * 128:(c4 + jj + 1) * 128], identity=identf.t[:])
                          op("act", "activation", reads=[pb.r], writes=[actT.r], out=actT.t[:, c4:c4 + n, s2 * 128:(s2 + 1) * 128], in_=pb.t[:, 0:n * 128].rearrange("p (c t) -> p c t", t=128), func=AF.Copy)
                      load_rows(x_src, r_src, g2 * 2 + s2, hrow[s2])
                  for cb in range(OB):
                      wb = wblk[cnt["w"] % 2]
                      cnt["w"] += 1
                      dma("poolq", reads=[r_in], writes=[wb.r], out=wb.t[:, :, 0:ncol], in_=wo[:, cb * 512:cb * 512 + ncol].rearrange("(c p) n -> p c n", p=128))
                      for s2 in range(2):
                          pa = P[2 + cnt["p"] % 2]
                          cnt["p"] += 1
                          for c in range(DC):
                              op("pe", "matmul", reads=[actT.r, wb.r], writes=[pa.r], out=pa.t[:, 0:ncol], lhsT=actT.t[:, c, s2 * 128:(s2 + 1) * 128], rhs=wb.t[:, c, 0:ncol], start=(c == 0), stop=(c == DC - 1))
                          hs = hrow[s2].t[:, cb * 512:cb * 512 + ncol]
                          op("dve", "scalar_tensor_tensor", reads=[hrow[s2].r, pa.r], writes=[hrow[s2].r], out=hs, in0=hs, scalar=alpha, in1=pa.t[:, 0:ncol], op0=ALU.mult, op1=ALU.add)
                  for s2 in range(2):
                      layer_norm_rows(hrow[s2], gbc, bbc, X1, r_X1, g2 * 2 + s2)
              Sc.barrier()
              if stop == 'C' + str(L):
                  raise _Stop()

          NBLK = (S * TOPK) // 128 + E
          PR = NBLK * 128
          FCB = FC * 128
          out_ap, r_o = (y_out, r_y) if L == depth - 1 else (X2, r_X2)
          with ExitStack() as esD:
              W4all = sb("W4all", [128, NT, 4], F32, esD)
              D4all = sb("D4all", [128, NT, 4], I32, esD)
              ebf = sb("ebf", [128, NBLK], F32, esD)
              iot6 = sb("iot6", [128, FC], F32, esD)
              iotp = sb("iotp", [128, 1], F32, esD)
              ioti = sb("ioti", [128, FC], I32, esD)
              op("pool", "iota", writes=[ioti.r], out=ioti.t[:], pattern=[[128, FC]], base=0, channel_multiplier=1)
              op("dve", "tensor_copy", reads=[ioti.r], writes=[iot6.r], out=iot6.t[:], in_=ioti.t[:])
              op("dve", "tensor_copy", reads=[ioti.r], writes=[iotp.r], out=iotp.t[:], in_=ioti.t[:, 0:1])
              with ExitStack() as es:
                  wtmp = [sb("wtmp", [128, DC, 128], BF16, es) for i in range(2)]
                  wdt = [sb("wdt", [128, FC, D], BF16, es) for i in range(2)]
                  zrow = sb("zrow", [128, D], F32, es)
                  bgT = sb("bgT", [128, E * FC], F32, es)
                  brow = sb("brow", [E, F], F32, es)
                  op("dve", "memset", writes=[zrow.r], ap=zrow.t[:], constant=0.0)
                  for b in range(NBLK):
                      for ch in range(NCH):
                          dma("sp", reads=[zrow.r], writes=[r_XS], out=XS[ch][b * 128:(b + 1) * 128, :], in_=zrow.t[:, ch * CW:(ch + 1) * CW])
                  k = 0
                  for e in range(E):
                      for (w_in_, W2) in ((wg_in, WG2), (wu_in, WU2)):
                          for fc in range(FC):
                              wt = wtmp[k % 2]
                              k += 1
                              dma("poolq", reads=[r_in], writes=[wt.r], out=wt.t[:], in_=w_in_[L, e][:, fc * 128:(fc + 1) * 128].rearrange("(c p) f -> p c f", p=128))
                              r0 = (e * FC + fc) * 128
                              dma("sp", reads=[wt.r], writes=[r_W2], out=W2[r0:r0 + 128, :], in_=wt.t[:].rearrange("p c f -> p (c f)"))
                      wd_ = wdt[e % 2]
                      dma("poolq", reads=[r_in], writes=[wd_.r], out=wd_.t[:], in_=wd_in[L, e].rearrange("(c p) n -> p c n", p=128))
                      dma("sp", reads=[wd_.r], writes=[r_W2], out=WD2[e * FCB:(e + 1) * FCB, :].rearrange("(c p) n -> p c n", p=128), in_=wd_.t[:])
                  for (b_in_, B2) in ((bg_in, BG2), (bu_in, BU2)):
                      dma("sp", reads=[r_in], writes=[brow.r], out=brow.t[:], in_=b_in_[L])
                      bT3 = bgT.t[:].rearrange("p (e c) -> p e c", c=FC)
                      for fc in range(FC):
                          op("pe", "transpose", reads=[brow.r, identf.r], writes=[P[5].r], out=P[5].t[:, 0:E], in_=brow.t[:, fc * 128:(fc + 1) * 128], identity=identf.t[0:E, 0:E])
                          op("act", "activation", reads=[P[5].r], writes=[bgT.r], out=bT3[:, :, fc], in_=P[5].t[:, 0:E], func=AF.Copy)
                      dma("sp", reads=[bgT.r], writes=[r_W2], out=B2.rearrange("(e p) c -> p e c", p=128), in_=bT3)
                  Sc.barrier()
              with ExitStack() as es:
                  Mall = sb("Mall", [128, NT, E], F32, es)
                  Wall = sb("Wall", [128, NT, E], F32, es)
                  Rall = sb("Rall", [128, NT, E], F32, es)
                  Bs = sb("Bs", [128, E], F32, es)
                  utri = sb("utri", [128, 128], BF16, es)
                  ones = sb("ones", [128, 128], BF16, es)
                  iou = sb("iou", [128, 128], I32, es)
                  xrow = [sb("xrow", [128, D], F32, es) for i in range(2)]
                  rw = sb("rw", [128, DC, E], F32, es)
                  rwh = sb("rwh", [128, DC, E], BF16, es)
                  rwl = sb("rwl", [128, DC, E], BF16, es)
                  rbb = sb("rbb", [128, E], F32, es)
                  xtf = [sb("xtf", [128, 128], F32, es) for i in range(2)]
                  xhi = [sb("xhi", [128, 128], BF16, es) for i in range(2)]
                  xlo = [sb("xlo", [128, 128], BF16, es) for i in range(2)]
                  logit = sb("logit", [128, E], F32, es)
                  mb = sb("mb", [128, E], BF16, es)
                  esum = sb("esum", [128, 1], F32, es)
                  cnti = sb("cnti", [128, E], I32, es)
                  padf = sb("padf", [128, E], F32, es)
                  pend = sb("pend", [128, E], F32, es)
                  pstart = sb("pstart", [128, E], F32, es)
                  tmpE = sb("tmpE", [128, E], F32, es)
                  key = sb("key", [128, E], F32, es)
                  d4f = sb("d4f", [128, 4], F32, es)
                  op("pool", "iota", writes=[iou.r], out=iou.t[:], pattern=[[1, 128]], base=0, channel_multiplier=-1)
                  op("dve", "tensor_scalar", reads=[iou.r], writes=[utri.r], out=utri.t[:], in0=iou.t[:], scalar1=0, scalar2=None, op0=ALU.is_gt)
                  op("dve", "memset", writes=[ones.r], ap=ones.t[:], constant=1.0)
                  op("dve", "memset", writes=[Bs.r], ap=Bs.t[:], constant=0.0)
                  dma("sp", reads=[r_in], writes=[rw.r], out=rw.t[:], in_=router_w[L].rearrange("(c p) e -> p c e", p=128))
                  op("dve", "tensor_copy", reads=[rw.r], writes=[rwh.r], out=rwh.t[:], in_=rw.t[:])
                  op("dve", "tensor_tensor", reads=[rw.r, rwh.r], writes=[rwl.r], out=rwl.t[:], in0=rw.t[:], in1=rwh.t[:], op=ALU.subtract)
                  dma("sp", reads=[r_in], writes=[rbb.r], out=rbb.t[:], in_=bc(router_b[L:L + 1, :]))
                  for tt in range(NT):
                      xb = xrow[tt % 2]
                      load_rows(X1, r_X1, tt, xb)
                      pl = P[6]
                      for c in range(DC):
                          pb = P[cnt["p"] % 2]
                          cnt["p"] += 1
                          op("pe", "transpose", reads=[xb.r, identf.r], writes=[pb.r], out=pb.t[:, 0:128], in_=xb.t[:, c * 128:(c + 1) * 128], identity=identf.t[:])
                          xf, xh, xl = xtf[c % 2], xhi[c % 2], xlo[c % 2]
                          op("act", "activation", reads=[pb.r], writes=[xf.r], out=xf.t[:], in_=pb.t[:, 0:128], func=AF.Copy)
                          op("dve", "tensor_copy", reads=[xf.r], writes=[xh.r], out=xh.t[:], in_=xf.t[:])
                          op("dve", "tensor_tensor", reads=[xf.r, xh.r], writes=[xl.r], out=xl.t[:], in0=xf.t[:], in1=xh.t[:], op=ALU.subtract)
                          op("pe", "matmul", reads=[xh.r, rwh.r], writes=[pl.r], out=pl.t[:, 0:E], lhsT=xh.t[:], rhs=rwh.t[:, c, :], start=(c == 0), stop=False)
                          op("pe", "matmul", reads=[xh.r, rwl.r], writes=[pl.r], out=pl.t[:, 0:E], lhsT=xh.t[:], rhs=rwl.t[:, c, :], start=False, stop=False)
                          op("pe", "matmul", reads=[xl.r, rwh.r], writes=[pl.r], out=pl.t[:, 0:E], lhsT=xl.t[:], rhs=rwh.t[:, c, :], start=False, stop=(c == DC - 1))
                      Mt = Mall.t[:, tt, :]
                      Wt = Wall.t[:, tt, :]
                      op("act", "activation", reads=[pl.r], writes=[logit.r], out=logit.t[:], in_=pl.t[:, 0:E], func=AF.Copy)
                      op("dve", "tensor_tensor", reads=[logit.r, rbb.r], writes=[logit.r], out=logit.t[:], in0=logit.t[:], in1=rbb.t[:], op=ALU.add)
                      op("dve", "max", reads=[logit.r], writes=[m8.r], out=m8.t[:], in_=logit.t[:])
                      op("dve", "tensor_scalar", reads=[logit.r, m8.r], writes=[Mall.r], out=Mt, in0=logit.t[:], scalar1=m8.t[:, TOPK - 1:TOPK], scalar2=None, op0=ALU.is_ge)
                      op("dve", "tensor_scalar", reads=[logit.r, m8.r], writes=[logit.r], out=logit.t[:], in0=logit.t[:], scalar1=m8.t[:, 0:1], scalar2=None, op0=ALU.subtract)
                      op("act", "activation", reads=[logit.r], writes=[logit.r], out=logit.t[:], in_=logit.t[:], func=AF.Exp)
                      op("dve", "tensor_tensor", reads=[logit.r, Mall.r], writes=[Wall.r], out=Wt, in0=logit.t[:], in1=Mt, op=ALU.mult)
                      op("dve", "tensor_reduce", reads=[Wall.r], writes=[esum.r], out=esum.t[:], in_=Wt, axis=AX.X, op=ALU.add)
                      op("dve", "reciprocal", reads=[esum.r], writes=[esum.r], out=esum.t[:], in_=esum.t[:])
                      op("dve", "tensor_scalar", reads=[Wall.r, esum.r], writes=[Wall.r], out=Wt, in0=Wt, scalar1=esum.t[:, 0:1], scalar2=None, op0=ALU.mult)
                      op("dve", "tensor_copy", reads=[Mall.r], writes=[mb.r], out=mb.t[:], in_=Mt)
                      pr, pc = P[2], P[3]
                      op("pe", "matmul", reads=[utri.r, mb.r], writes=[pr.r], out=pr.t[:, 0:E], lhsT=utri.t[:], rhs=mb.t[:], start=True, stop=True)
                      op("pe", "matmul", reads=[ones.r, mb.r], writes=[pc.r], out=pc.t[:, 0:E], lhsT=ones.t[:], rhs=mb.t[:], start=True, stop=True)
                      op("dve", "tensor_tensor", reads=[pr.r, Bs.r], writes=[Rall.r], out=Rall.t[:, tt, :], in0=pr.t[:, 0:E], in1=Bs.t[:], op=ALU.add)
                      op("dve", "tensor_tensor", reads=[pc.r, Bs.r], writes=[Bs.r], out=Bs.t[:], in0=pc.t[:, 0:E], in1=Bs.t[:], op=ALU.add)
                  op("dve", "tensor_scalar", reads=[Bs.r], writes=[cnti.r], out=cnti.t[:], in0=Bs.t[:], scalar1=127.0, scalar2=None, op0=ALU.add)
                  op("dve", "tensor_scalar", reads=[cnti.r], writes=[cnti.r], out=cnti.t[:], in0=cnti.t[:], scalar1=7, scalar2=None, op0=ALU.arith_shift_right)
                  op("dve", "tensor_scalar", reads=[cnti.r], writes=[cnti.r], out=cnti.t[:], in0=cnti.t[:], scalar1=7, scalar2=None, op0=ALU.logical_shift_left)
                  op("dve", "tensor_copy", reads=[cnti.r], writes=[padf.r], out=padf.t[:], in_=cnti.t[:])
                  op("dve", "tensor_copy", reads=[padf.r], writes=[pend.r], out=pend.t[:], in_=padf.t[:])
                  for e in range(1, E):
                      op("dve", "tensor_tensor", reads=[pend.r], writes=[pend.r], out=pend.t[:, e:e + 1], in0=pend.t[:, e:e + 1], in1=pend.t[:, e - 1:e], op=ALU.add)
                  op("dve", "tensor_tensor", reads=[pend.r, padf.r], writes=[pstart.r], out=pstart.t[:], in0=pend.t[:], in1=padf.t[:], op=ALU.subtract)
                  for b in range(NBLK):
                      op("dve", "tensor_scalar", reads=[pend.r], writes=[tmpE.r, ebf.r], out=tmpE.t[:], in0=pend.t[:], scalar1=float(128 * b), scalar2=0.0, op0=ALU.is_le, op1=ALU.add, accum_out=ebf.t[:, b:b + 1])
                  op("dve", "tensor_scalar", reads=[ebf.r], writes=[ebf.r], out=ebf.t[:], in0=ebf.t[:], scalar1=float(E - 1), scalar2=None, op0=ALU.min)
                  Sc.barrier()
                  for tt in range(NT):
                      xb = xrow[tt % 2]
                      load_rows(X1, r_X1, tt, xb)
                      Mt = Mall.t[:, tt, :]
                      Wt = Wall.t[:, tt, :]
                      op("dve", "tensor_tensor", reads=[Rall.r, pstart.r], writes=[key.r], out=key.t[:], in0=Rall.t[:, tt, :], in1=pstart.t[:], op=ALU.add)
                      op("dve", "scalar_tensor_tensor", reads=[key.r, Mall.r], writes=[key.r], out=key.t[:], in0=key.t[:], scalar=1.0, in1=Mt, op0=ALU.add, op1=ALU.mult)
                      op("dve", "max", reads=[key.r], writes=[m8.r], out=m8.t[:], in_=key.t[:])
                      op("dve", "tensor_scalar", reads=[m8.r], writes=[d4f.r], out=d4f.t[:], in0=m8.t[:, 0:4], scalar1=-1.0, scalar2=None, op0=ALU.add)
                      op("dve", "tensor_copy", reads=[d4f.r], writes=[D4all.r], out=D4all.t[:, tt, :], in_=d4f.t[:])
                      for k in range(4):
                          op("dve", "tensor_scalar", reads=[key.r, m8.r], writes=[tmpE.r], out=tmpE.t[:], in0=key.t[:], scalar1=m8.t[:, k:k + 1], scalar2=None, op0=ALU.is_equal)
                          op("dve", "tensor_tensor", reads=[tmpE.r, Wall.r], writes=[tmpE.r], out=tmpE.t[:], in0=tmpE.t[:], in1=Wt, op=ALU.mult)
                          op("dve", "tensor_reduce", reads=[tmpE.r], writes=[W4all.r], out=W4all.t[:, tt, k:k + 1], in_=tmpE.t[:], axis=AX.X, op=ALU.add)
                      for k in range(4):
                          for ch in range(NCH):
                              Sc.dma_fn("poolq", "indirect_dma_start", reads=[xb.r, D4all.r], writes=[r_XS], out=XS[ch][:, :], out_offset=bass.IndirectOffsetOnAxis(ap=D4all.t[:, tt, k:k + 1], axis=0), in_=xb.t[:, ch * CW:(ch + 1) * CW], in_offset=None)
                  Sc.barrier()
              with ExitStack() as es:
                  xrow = [sb("xrow", [128, D], F32, es) for i in range(2)]
                  xT = sb("xT", [128, DC, 128], BF16, es)
                  wgb = [sb("wgb", [128, DC * 128], BF16, es) for i in range(2)]
                  wub = [sb("wub", [128, DC * 128], BF16, es) for i in range(2)]
                  wdb = sb("wdb", [128, FC, D], BF16, es)
                  yrow = sb("yrow", [128, D], F32, es)
                  bdrow = sb("bdrow", [128, D], F32, es)
                  bgb = sb("bgb", [128, FC], F32, es)
                  bub = sb("bub", [128, FC], F32, es)
                  idx6 = [sb("idx6", [128, FC], I32, es) for i in range(2)]
                  idxb = [sb("idxb", [128, 1], I32, es) for i in range(2)]
                  idxe = [sb("idxe", [128, 1], I32, es) for i in range(2)]
                  gt = sb("gt", [128, 128], F32, es)
                  sg = sb("sg", [128, 128], F32, es)
                  ut = sb("ut", [128, 128], F32, es)
                  hT = sb("hT", [128, FC, 128], BF16, es)
                  for b in range(NBLK):
                      i6, ib, ie = idx6[b % 2], idxb[b % 2], idxe[b % 2]
                      eb = ebf.t[:, b:b + 1]
                      for fc in range(FC - 1, -1, -1):
                          op("dve", "tensor_scalar", reads=[ebf.r, iot6.r, i6.r], writes=[i6.r], out=i6.t[:, fc:fc + 1], in0=eb, scalar1=float(FCB), scalar2=iot6.t[:, fc:fc + 1], op0=ALU.mult, op1=ALU.add)
                      op("dve", "tensor_scalar", reads=[ebf.r, iotp.r], writes=[ib.r], out=ib.t[:], in0=eb, scalar1=128.0, scalar2=iotp.t[:, 0:1], op0=ALU.mult, op1=ALU.add)
                      op("dve", "tensor_scalar", reads=[ebf.r], writes=[ie.r], out=ie.t[:], in0=eb, scalar1=float(L * E), scalar2=None, op0=ALU.add)
                      xb = xrow[b % 2]
                      for ch in range(NCH):
                          dma("sp", reads=[r_XS], writes=[xb.r], out=xb.t[:, ch * CW:(ch + 1) * CW], in_=XS[ch][b * 128:(b + 1) * 128, :])
                      for c4 in range(0, DC, 4):
                          pb = P[cnt["p"] % 2]
                          cnt["p"] += 1
                          n = min(4, DC - c4)
                          for jj in range(n):
                              op("pe", "transpose", reads=[xb.r, identf.r], writes=[pb.r], out=pb.t[:, jj * 128:(jj + 1) * 128], in_=xb.t[:, (c4 + jj) * 128:(c4 + jj + 1) * 128], identity=identf.t[:])
                          src = pb.t[:, 0:n * 128].rearrange("p (c t) -> p c t", t=128)
                          if (c4 // 4) % 2 == 0:
                              op("act", "activation", reads=[pb.r], writes=[xT.r], out=xT.t[:, c4:c4 + n, :], in_=src, func=AF.Copy)
                          else:
                              op("dve", "tensor_copy", reads=[pb.r], writes=[xT.r], out=xT.t[:, c4:c4 + n, :], in_=src)
                      Sc.dma_fn("poolq", "indirect_dma_start", reads=[r_W2, ib.r], writes=[bgb.r], out=bgb.t[:], out_offset=None, in_=BG2[:, :], in_offset=bass.IndirectOffsetOnAxis(ap=ib.t[:, 0:1], axis=0))
                      Sc.dma_fn("poolq", "indirect_dma_start", reads=[r_W2, ib.r], writes=[bub.r], out=bub.t[:], out_offset=None, in_=BU2[:, :], in_offset=bass.IndirectOffsetOnAxis(ap=ib.t[:, 0:1], axis=0))
                      Sc.dma_fn("poolq", "indirect_dma_start", reads=[r_in, ie.r], writes=[bdrow.r], out=bdrow.t[:], out_offset=None, in_=bd_in.rearrange("l e d -> (l e) d"), in_offset=bass.IndirectOffsetOnAxis(ap=ie.t[:, 0:1], axis=0))
                      for fc in range(FC):
                          Sc.dma_fn("poolq", "indirect_dma_start", reads=[r_W2, i6.r], writes=[wdb.r], out=wdb.t[:, fc, :], out_offset=None, in_=WD2[:, :], in_offset=bass.IndirectOffsetOnAxis(ap=i6.t[:, fc:fc + 1], axis=0))
                      for fc in range(FC):
                          wg_, wu_ = wgb[fc % 2], wub[fc % 2]
                          Sc.dma_fn("poolq", "indirect_dma_start", reads=[r_W2, i6.r], writes=[wg_.r], out=wg_.t[:], out_offset=None, in_=WG2[:, :], in_offset=bass.IndirectOffsetOnAxis(ap=i6.t[:, fc:fc + 1], axis=0))
                          Sc.dma_fn("poolq", "indirect_dma_start", reads=[r_W2, i6.r], writes=[wu_.r], out=wu_.t[:], out_offset=None, in_=WU2[:, :], in_offset=bass.IndirectOffsetOnAxis(ap=i6.t[:, fc:fc + 1], axis=0))
                          pgt, put = P[2], P[3]
                          for c in range(DC):
                              op("pe", "matmul", reads=[wg_.r, xT.r], writes=[pgt.r], out=pgt.t[:, 0:128], lhsT=wg_.t[:, c * 128:(c + 1) * 128], rhs=xT.t[:, c, :], start=(c == 0), stop=(c == DC - 1))
                          for c in range(DC):
                              op("pe", "matmul", reads=[wu_.r, xT.r], writes=[put.r], out=put.t[:, 0:128], lhsT=wu_.t[:, c * 128:(c + 1) * 128], rhs=xT.t[:, c, :], start=(c == 0), stop=(c == DC - 1))
                          op("dve", "tensor_scalar", reads=[pgt.r, bgb.r], writes=[gt.r], out=gt.t[:], in0=pgt.t[:, 0:128], scalar1=bgb.t[:, fc:fc + 1], scalar2=float(cfg["limit"]), op0=ALU.add, op1=ALU.min)
                          op("act", "activation", reads=[gt.r], writes=[sg.r], out=sg.t[:], in_=gt.t[:], func=AF.Sigmoid, scale=float(cfg["swiglu_alpha"]))
                          op("dve", "tensor_scalar", reads=[put.r, bub.r], writes=[ut.r], out=ut.t[:], in0=put.t[:, 0:128], scalar1=bub.t[:, fc:fc + 1], scalar2=float(cfg["limit"]), op0=ALU.add, op1=ALU.min)
                          op("dve", "tensor_scalar", reads=[ut.r], writes=[ut.r], out=ut.t[:], in0=ut.t[:], scalar1=-float(cfg["limit"]), scalar2=1.0, op0=ALU.max, op1=ALU.add)
                          op("dve", "tensor_tensor", reads=[gt.r, sg.r], writes=[gt.r], out=gt.t[:], in0=gt.t[:], in1=sg.t[:], op=ALU.mult)
                          op("dve", "tensor_tensor", reads=[gt.r, ut.r], writes=[hT.r], out=hT.t[:, fc, :], in0=gt.t[:], in1=ut.t[:], op=ALU.mult)
                      for cb in range(OB):
                          pa = P[4 + cb % 2]
                          for fc in range(FC):
                              op("pe", "matmul", reads=[hT.r, wdb.r], writes=[pa.r], out=pa.t[:, 0:ncol], lhsT=hT.t[:, fc, :], rhs=wdb.t[:, fc, cb * 512:cb * 512 + ncol], start=(fc == 0), stop=(fc == FC - 1))
                          op("dve", "tensor_tensor", reads=[pa.r, bdrow.r], writes=[yrow.r], out=yrow.t[:, cb * 512:cb * 512 + ncol], in0=pa.t[:, 0:ncol], in1=bdrow.t[:, cb * 512:cb * 512 + ncol], op=ALU.add)
                      for ch in range(NCH):
                          dma("sp", reads=[yrow.r], writes=[r_YS], out=YS[ch][b * 128:(b + 1) * 128, :], in_=yrow.t[:, ch * CW:(ch + 1) * CW])
                  Sc.barrier()
              with ExitStack() as es:
                  gbc = sb("gbc", [128, D], F32, es)
                  bbc = sb("bbc", [128, D], F32, es)
                  hrow = [sb("hrow", [128, D], F32, es) for i in range(2)]
                  grow = [sb("grow", [128, D], F32, es) for i in range(4)]
                  dma("sp", reads=[r_in], writes=[gbc.r], out=gbc.t[:], in_=bc(ln_ffn_g[L:L + 1, :]))
                  dma("sp", reads=[r_in], writes=[bbc.r], out=bbc.t[:], in_=bc(ln_ffn_b[L:L + 1, :]))
                  for tt in range(NT):
                      hb = hrow[tt % 2]
                      load_rows(X1, r_X1, tt, hb)
                      for k in range(4):
                          for ch in range(NCH):
                              Sc.dma_fn("poolq", "indirect_dma_start", reads=[r_YS, D4all.r], writes=[grow[k].r], out=grow[k].t[:, ch * CW:(ch + 1) * CW], out_offset=None, in_=YS[ch][:, :], in_offset=bass.IndirectOffsetOnAxis(ap=D4all.t[:, tt, k:k + 1], axis=0))
                      op("dve", "tensor_scalar", reads=[hb.r], writes=[hb.r], out=hb.t[:], in0=hb.t[:], scalar1=alpha, scalar2=None, op0=ALU.mult)
                      for k in range(4):
                          op("dve", "scalar_tensor_tensor", reads=[grow[k].r, W4all.r, hb.r], writes=[hb.r], out=hb.t[:], in0=grow[k].t[:], scalar=W4all.t[:, tt, k:k + 1], in1=hb.t[:], op0=ALU.mult, op1=ALU.add)
                      layer_norm_rows(hb, gbc, bbc, out_ap, r_o, tt)
                  Sc.barrier()
              if stop == 'D' + str(L):
                  raise _Stop()


    except _Stop:
        dsrc = {'X1': (X1, r_X1), 'X2': (X2, r_X2), 'AO': (AO, r_AO)}.get(cfg.get('dump'))
        if dsrc is not None:
            for r0 in range(0, S, 512):
                dma("sp", reads=[dsrc[1]], writes=[r_y], out=y_out[r0:r0 + 512, :], in_=dsrc[0][r0:r0 + 512, :])
    Sc.wait_all("sp", [r_y.w] if r_y.w else [])
    fin = [(ent[0], ent[1]) for q in Sc.dma_sems for ent in Sc.dma_sems[q] if ent[1] > 0]
    Sc.wait_all("sp", fin)
    Sc.emit()
    return nc


FULL_CFG = dict(D=4096, S=8192, E=32, F=768, depth=2, topk=4, theta=500000.0,
                ln_eps=1e-5, rms_eps=1e-5, limit=7.0, swiglu_alpha=1.702)

_NAMES = ["diff_w_qkv", "diff_lambda_q1", "diff_lambda_k1", "diff_lambda_q2", "diff_lambda_k2",
          "diff_subln_g", "diff_w_o", "moba_w_qkv", "moba_w_o", "ln_mix_g", "ln_mix_b", "router_w",
          "router_b", "exp_w_gate", "exp_b_gate", "exp_w_up", "exp_b_up", "exp_w_down", "exp_b_down",
          "ln_ffn_g", "ln_ffn_b"]


def run_cfg(cfg, inputs):
    nc = build_program(cfg)
    B = inputs["x"].shape[0]
    in_maps = []
    for b in range(B):
        m = {"x": np.ascontiguousarray(inputs["x"][b], dtype=np.float32),
             "positions": np.ascontiguousarray(np.asarray(inputs["positions"][b]).reshape(-1, 1), dtype=np.int32)}
        for n in _NAMES:
            m[n] = np.ascontiguousarray(inputs[n], dtype=np.float32)
        in_maps.append(m)
    res = run_bass_kernel_spmd(nc, in_maps, core_ids=list(range(B)))
    return np.stack([np.asarray(r["y"]) for r in res.results], axis=0).astype(np.float32)


def kernel(**inputs):
    inputs = {k: np.asarray(v) for k, v in inputs.items()}
    return run_cfg(FULL_CFG, inputs)
```

```python
import math
import numpy as np
import concourse.bass as bass
import concourse.mybir as mybir
from concourse.bass_utils import run_bass_kernel_spmd

F32 = mybir.dt.float32
BF16 = mybir.dt.bfloat16
I32 = mybir.dt.int32
ALU = mybir.AluOpType
AF = mybir.ActivationFunctionType
AX = mybir.AxisListType
SEM_ROT = 100000
NEG = -30000.0


class Res:
    __slots__ = ("w", "r")

    def __init__(self):
        self.w = None
        self.r = []


class Sched:
    def __init__(self, nc, n_dma_sems=16):
        self.nc = nc
        self.lists = {e: [] for e in ("pe", "dve", "act", "pool", "sp")}
        self.cur_sem = {}
        self.cur_cnt = {}
        for e in ("pe", "dve", "act", "pool"):
            self._new_sem(e)
        self.seen = {e: {} for e in self.lists}
        self.dma_sems = {}
        self.dma_idx = {}
        for q in ("sp", "poolq"):
            self.dma_sems[q] = [[nc.alloc_semaphore(name=f"d_{q}_{i}"), 0] for i in range(n_dma_sems)]
            self.dma_idx[q] = 0
        self.n_ops = 0

    def _new_sem(self, e):
        self.cur_sem[e] = self.nc.alloc_semaphore(name=f"s_{e}_{len(self.lists[e])}")
        self.cur_cnt[e] = 0

    @staticmethod
    def _deps(reads, writes):
        deps = {}

        def add(tok):
            if tok is None:
                return
            s, v = tok
            k = id(s)
            if k not in deps or deps[k][1] < v:
                deps[k] = (s, v)
        for r in reads:
            add(r.w)
        for w in writes:
            add(w.w)
            for t in w.r:
                add(t)
        return deps

    def _finish(self, st, deps, fn, tok, inc, reads, writes, skip_sem=None):
        waits = []
        for k, (s, v) in deps.items():
            if skip_sem is not None and s is skip_sem:
                continue
            if self.seen[st].get(k, 0) >= v:
                continue
            self.seen[st][k] = v
            waits.append((s, v))
        self.lists[st].append((waits, fn, tok[0], inc))
        for r in reads:
            r.r.append(tok)
            if len(r.r) > 64:
                r.r = r.r[-64:] if False else r.r
        for w in writes:
            w.w = tok
            w.r = []
        self.n_ops += 1
        return tok

    def op(self, eng, name, reads=(), writes=(), **kw):
        deps = self._deps(reads, writes)
        if self.cur_cnt[eng] >= SEM_ROT:
            self._new_sem(eng)
        self.cur_cnt[eng] += 1
        tok = (self.cur_sem[eng], self.cur_cnt[eng])
        fn = (lambda e, name=name, kw=kw: getattr(e, name)(**kw))
        return self._finish(eng, deps, fn, tok, 1, reads, writes,
                            skip_sem=self.cur_sem["pe"] if eng == "pe" else None)

    def dma(self, q, reads=(), writes=(), **kw):
        st = "pool" if q == "poolq" else "sp"
        deps = self._deps(reads, writes)
        pool = self.dma_sems[q]
        i = self.dma_idx[q]
        self.dma_idx[q] = (i + 1) % len(pool)
        ent = pool[i]
        if ent[1] > 0:
            k = id(ent[0])
            if k not in deps or deps[k][1] < ent[1]:
                deps[k] = (ent[0], ent[1])
        ent[1] += 16
        tok = (ent[0], ent[1])
        fn = (lambda e, kw=kw: e.dma_start(**kw))
        return self._finish(st, deps, fn, tok, 16, reads, writes)

    def dma_fn(self, q, name, reads=(), writes=(), **kw):
        st = "pool" if q == "poolq" else "sp"
        deps = self._deps(reads, writes)
        pool = self.dma_sems[q]
        i = self.dma_idx[q]
        self.dma_idx[q] = (i + 1) % len(pool)
        ent = pool[i]
        if ent[1] > 0:
            k = id(ent[0])
            if k not in deps or deps[k][1] < ent[1]:
                deps[k] = (ent[0], ent[1])
        ent[1] += 16
        tok = (ent[0], ent[1])
        fn = (lambda e, name=name, kw=kw: getattr(e, name)(**kw))
        return self._finish(st, deps, fn, tok, 16, reads, writes)

    def barrier(self):
        toks = [(self.cur_sem[e], self.cur_cnt[e]) for e in ("pe", "dve", "act", "pool") if self.cur_cnt[e] > 0]
        toks += [(ent[0], ent[1]) for q in self.dma_sems for ent in self.dma_sems[q] if ent[1] > 0]
        for st in self.lists:
            waits = []
            for (s_, v) in toks:
                if self.seen[st].get(id(s_), 0) >= v:
                    continue
                self.seen[st][id(s_)] = v
                waits.append((s_, v))
            self.lists[st].append((waits, None, None, 0))

    def wait_all(self, eng, toks):
        self.lists[eng].append(([(s, v) for (s, v) in toks], None, None, 0))

    def emit(self):
        nc = self.nc
        lists = self.lists
        with nc.Block() as block:
            def run(engine, items):
                for waits, fn, sem, inc in items:
                    for (s, v) in waits:
                        engine.wait_ge(s, v)
                    if fn is not None:
                        fn(engine).then_inc(sem, inc)

            @block.tensor
            def _(e):
                run(e, lists["pe"])

            @block.vector
            def _(e):
                run(e, lists["dve"])

            @block.scalar
            def _(e):
                run(e, lists["act"])

            @block.gpsimd
            def _(e):
                run(e, lists["pool"])

            @block.sync
            def _(e):
                run(e, lists["sp"])


class Buf:
    def __init__(self, t):
        self.t = t
        self.r = Res()


def build_program(cfg):
    D, S, E, F, depth = cfg["D"], cfg["S"], cfg["E"], cfg["F"], cfg["depth"]
    TOPK = cfg["topk"]
    theta = cfg["theta"]
    DC = D // 128
    NT = S // 128
    NG = S // 512
    NM = D // 128
    FC = F // 128
    NB = S // 256
    NBP = max(NB, 8)
    alpha = float((2 * depth) ** 0.25)
    n_diff = (depth + 1) // 2
    n_moba = depth // 2
    scale = float(128 ** -0.5)

    nc = bass.Bass("TRN2", target_bir_lowering=False)
    ext = {}

    def din(name, shape, dt=F32):
        ext[name] = nc.dram_tensor(name, list(shape), dt, kind="ExternalInput").ap()
        return ext[name]
    x_in = din("x", [S, D])
    pos_in = din("positions", [S, 1], I32)
    diff_wqkv = din("diff_w_qkv", [n_diff, D, 3 * D])
    lq1 = din("diff_lambda_q1", [n_diff, 128]); lk1 = din("diff_lambda_k1", [n_diff, 128])
    lq2 = din("diff_lambda_q2", [n_diff, 128]); lk2 = din("diff_lambda_k2", [n_diff, 128])
    subg = din("diff_subln_g", [n_diff, 256])
    diff_wo = din("diff_w_o", [n_diff, D, D])
    moba_wqkv = din("moba_w_qkv", [max(n_moba, 1), D, 3 * D])
    moba_wo = din("moba_w_o", [max(n_moba, 1), D, D])
    ln_mix_g = din("ln_mix_g", [depth, D]); ln_mix_b = din("ln_mix_b", [depth, D])
    router_w = din("router_w", [depth, D, E]); router_b = din("router_b", [depth, E])
    wg_in = din("exp_w_gate", [depth, E, D, F]); bg_in = din("exp_b_gate", [depth, E, F])
    wu_in = din("exp_w_up", [depth, E, D, F]); bu_in = din("exp_b_up", [depth, E, F])
    wd_in = din("exp_w_down", [depth, E, F, D]); bd_in = din("exp_b_down", [depth, E, D])
    ln_ffn_g = din("ln_ffn_g", [depth, D]); ln_ffn_b = din("ln_ffn_b", [depth, D])
    y_out = nc.dram_tensor("y", [S, D], F32, kind="ExternalOutput").ap()

    def dscr(name, shape, dt):
        return nc.dram_tensor(name, list(shape), dt, kind="Internal").ap()
    QT = dscr("QT", [NM, 128, S], BF16); r_QT = Res()
    KT = dscr("KT", [NM, 128, S], BF16); r_KT = Res()
    Vd = dscr("Vd", [S, D], BF16); r_V = Res()
    AO = dscr("AO", [S, D], F32); r_AO = Res()
    X1 = dscr("X1", [S, D], F32); r_X1 = Res()
    X2 = dscr("X2", [S, D], F32); r_X2 = Res()
    r_in = Res()
    r_y = Res()
    GB = 256
    _NBLK = (S * TOPK) // GB + E
    CW = min(1024, D)
    NCH = D // CW
    XS = [dscr(f"XS{i}", [_NBLK * GB, CW], F32) for i in range(NCH)]; r_XS = Res()
    YS = [dscr(f"YS{i}", [_NBLK * GB, CW], F32) for i in range(NCH)]; r_YS = Res()
    _FC = F // 128
    NGF = _FC // 2
    WGJ = [dscr(f"WGJ{i}", [E * 128, 2 * D], BF16) for i in range(NGF)]
    WUJ = [dscr(f"WUJ{i}", [E * 128, 2 * D], BF16) for i in range(NGF)]
    WD2 = dscr("WD2", [E * 128, _FC * D], BF16)
    BGU2 = dscr("BGU2", [E * 128, 2 * _FC], F32)
    r_W2 = Res()

    Sc = Sched(nc)
    op, dma = Sc.op, Sc.dma

    uid = [0]

    def sb(name, shape, dt, es=None):
        if es is None:
            return Buf(nc.alloc_sbuf_tensor(name, list(shape), dt))
        uid[0] += 1
        return Buf(es.enter_context(nc.sbuf_tensor(f"{name}_{uid[0]}", list(shape), dt)))

    def bc(ap2d):
        return ap2d.broadcast_to([128, ap2d.shape[1]])
    P = [Buf(nc.alloc_psum_tensor(f"P{i}", [128, 512], F32)) for i in range(7)]
    PB = Buf(nc.alloc_psum_tensor("PB", [128, 1024], BF16))

    from contextlib import ExitStack
    identf = sb("identf", [128, 128], F32)
    identb = sb("identb", [128, 128], BF16)
    eoh = sb("eoh", [32, NBP * 128], BF16)
    cmask = [sb(f"cmask{i}", [128, 512], F32) for i in range(4)]
    sinT = sb("sinT", [128, NT * 16], F32)
    cosT = sb("cosT", [128, NT * 16], F32)
    m8 = sb("m8", [128, 8], F32)
    stats = sb("stats", [128, max(1, D // 512) * 6], F32)
    mv = sb("mv", [128, 2], F32)
    rstd = sb("rstd", [128, 1], F32)
    with ExitStack() as es:
        io = sb("io", [128, 128], I32, es)
        op("pool", "iota", writes=[io.r], out=io.t[:], pattern=[[1, 128]], base=0, channel_multiplier=-1)
        op("dve", "tensor_scalar", reads=[io.r], writes=[identf.r], out=identf.t[:], in0=io.t[:], scalar1=0, scalar2=None, op0=ALU.is_equal)
        op("dve", "tensor_copy", reads=[identf.r], writes=[identb.r], out=identb.t[:], in_=identf.t[:])
        io2 = sb("io2", [32, NBP * 128], I32, es)
        op("pool", "iota", writes=[io2.r], out=io2.t[:], pattern=[[1, NBP], [0, 128]], base=0, channel_multiplier=-1)
        op("dve", "tensor_scalar", reads=[io2.r], writes=[eoh.r], out=eoh.t[:], in0=io2.t[:], scalar1=0, scalar2=None, op0=ALU.is_equal)
        iom = sb("iom", [128, 512], I32, es)
        op("pool", "iota", writes=[iom.r], out=iom.t[:], pattern=[[1, 512]], base=0, channel_multiplier=-1)
        for i in range(4):
            op("dve", "tensor_scalar", reads=[iom.r], writes=[cmask[i].r], out=cmask[i].t[:], in0=iom.t[:], scalar1=128 * i, scalar2=None, op0=ALU.is_ge)
            op("dve", "tensor_scalar", reads=[cmask[i].r], writes=[cmask[i].r], out=cmask[i].t[:], in0=cmask[i].t[:], scalar1=-1.0, scalar2=-NEG, op0=ALU.add, op1=ALU.mult)
        posf = sb("posf", [128, NT], F32, es)
        ang = sb("ang", [128, NT * 16], F32, es)
        ki = sb("ki", [128, NT * 16], I32, es)
        kf = sb("kf", [128, NT * 16], F32, es)
        posr = sb("posr", [NT, 128], I32, es)
        posrf = sb("posrf", [NT, 128], F32, es)
        dma("sp", reads=[r_in], writes=[posr.r], out=posr.t[:], in_=pos_in.rearrange("(t p) o -> t (p o)", p=128))
        op("dve", "tensor_copy", reads=[posr.r], writes=[posrf.r], out=posrf.t[:], in_=posr.t[:])
        op("pe", "transpose", reads=[posrf.r, identf.r], writes=[P[0].r], out=P[0].t[:, 0:NT], in_=posrf.t[:], identity=identf.t[0:NT, 0:NT])
        op("act", "activation", reads=[P[0].r], writes=[posf.r], out=posf.t[:], in_=P[0].t[:, 0:NT], func=AF.Copy)
        inv = (np.float32(1.0) / (np.float32(theta) ** (np.arange(0, 32, 2, dtype=np.float32) / np.float32(32)))).astype(np.float32)
        ang3 = ang.t[:].rearrange("p (t i) -> p t i", i=16)
        for i in range(16):
            op("dve", "tensor_scalar", reads=[posf.r], writes=[ang.r], out=ang3[:, :, i], in0=posf.t[:], scalar1=float(inv[i]), scalar2=None, op0=ALU.mult)
        C1 = 6.28125
        C2 = float(2 * np.pi - 6.28125)
        PI = float(np.pi)

        def wrap(dst, src):
            op("dve", "tensor_scalar", reads=[src.r], writes=[kf.r], out=kf.t[:], in0=src.t[:], scalar1=PI, scalar2=-2 * PI, op0=ALU.is_gt, op1=ALU.mult)
            op("dve", "tensor_tensor", reads=[src.r, kf.r], writes=[dst.r], out=dst.t[:], in0=src.t[:], in1=kf.t[:], op=ALU.add)
            op("dve", "tensor_scalar", reads=[src.r], writes=[kf.r], out=kf.t[:], in0=src.t[:], scalar1=-PI, scalar2=2 * PI, op0=ALU.is_lt, op1=ALU.mult)
            op("dve", "tensor_tensor", reads=[dst.r, kf.r], writes=[dst.r], out=dst.t[:], in0=dst.t[:], in1=kf.t[:], op=ALU.add)
            op("dve", "tensor_scalar", reads=[dst.r], writes=[dst.r], out=dst.t[:], in0=dst.t[:], scalar1=PI, scalar2=-PI, op0=ALU.min, op1=ALU.max)
        op("dve", "tensor_scalar", reads=[ang.r], writes=[ki.r], out=ki.t[:], in0=ang.t[:], scalar1=float(1 / (2 * np.pi)), scalar2=None, op0=ALU.mult)
        op("dve", "tensor_copy", reads=[ki.r], writes=[kf.r], out=kf.t[:], in_=ki.t[:])
        op("dve", "scalar_tensor_tensor", reads=[kf.r, ang.r], writes=[ang.r], out=ang.t[:], in0=kf.t[:], scalar=-C1, in1=ang.t[:], op0=ALU.mult, op1=ALU.add)
        op("dve", "scalar_tensor_tensor", reads=[kf.r, ang.r], writes=[ang.r], out=ang.t[:], in0=kf.t[:], scalar=-C2, in1=ang.t[:], op0=ALU.mult, op1=ALU.add)
        wrap(sinT, ang)
        op("dve", "tensor_scalar", reads=[sinT.r], writes=[ang.r], out=ang.t[:], in0=sinT.t[:], scalar1=PI / 2, scalar2=None, op0=ALU.add)
        wrap(cosT, ang)
        op("act", "activation", reads=[sinT.r], writes=[sinT.r], out=sinT.t[:], in_=sinT.t[:], func=AF.Sin)
        op("act", "activation", reads=[cosT.r], writes=[cosT.r], out=cosT.t[:], in_=cosT.t[:], func=AF.Sin)
        Sc.barrier()
    sin3 = sinT.t[:].rearrange("p (t i) -> p t i", i=16)
    cos3 = cosT.t[:].rearrange("p (t i) -> p t i", i=16)
    cnt = {"w": 0, "p": 0, "st": 0, "ts": 0, "xr": 0}

    def load_rows(src_ap, r_src, tile_idx, buf):
        dma("sp", reads=[r_src], writes=[buf.r], out=buf.t[:], in_=src_ap[tile_idx * 128:(tile_idx + 1) * 128, :])

    def layer_norm_rows(hb, gbc, bbc, out_ap, r_out, tile_idx):
        nch = max(1, D // 512)
        w = D // nch
        for c in range(nch):
            op("dve", "bn_stats", reads=[hb.r], writes=[stats.r], out=stats.t[:, c * 6:(c + 1) * 6], in_=hb.t[:, c * w:(c + 1) * w])
        op("dve", "bn_aggr", reads=[stats.r], writes=[mv.r], out=mv.t[:], in_=stats.t[:, 0:nch * 6])
        op("dve", "tensor_scalar", reads=[mv.r], writes=[rstd.r], out=rstd.t[:], in0=mv.t[:, 1:2], scalar1=float(cfg["ln_eps"]), scalar2=None, op0=ALU.add)
        op("act", "activation", reads=[rstd.r], writes=[rstd.r], out=rstd.t[:], in_=rstd.t[:], func=AF.Sqrt)
        op("dve", "reciprocal", reads=[rstd.r], writes=[rstd.r], out=rstd.t[:], in_=rstd.t[:])
        op("dve", "tensor_scalar", reads=[hb.r, mv.r, rstd.r], writes=[hb.r], out=hb.t[:], in0=hb.t[:], scalar1=mv.t[:, 0:1], scalar2=rstd.t[:, 0:1], op0=ALU.subtract, op1=ALU.mult)
        op("dve", "tensor_tensor", reads=[hb.r, gbc.r], writes=[hb.r], out=hb.t[:], in0=hb.t[:], in1=gbc.t[:], op=ALU.mult)
        op("dve", "tensor_tensor", reads=[hb.r, bbc.r], writes=[hb.r], out=hb.t[:], in0=hb.t[:], in1=bbc.t[:], op=ALU.add)
        dma("sp", reads=[hb.r], writes=[r_out], out=out_ap[tile_idx * 128:(tile_idx + 1) * 128, :], in_=hb.t[:])

    QKB = 3 * D // 512
    class _Stop(Exception):
        pass
    stop = cfg.get('stop')
    try:
      if stop == 'setup':
          raise _Stop()
      for L in range(depth):
          is_diff = (L % 2 == 0)
          j = L // 2
          wqkv = (diff_wqkv if is_diff else moba_wqkv)[j]
          wo = (diff_wo if is_diff else moba_wo)[j]
          x_src, r_src = (x_in, r_in) if L == 0 else (X2, r_X2)
          lambda_init = 0.8 - 0.6 * math.exp(-0.3 * L)
          dv = 256 if is_diff else 128

          with ExitStack() as es:
              xrow = [sb("xrow", [128, D], F32, es) for i in range(2)]
              actT = sb("actT", [128, DC, 512], BF16, es)
              wblk = [sb("wblk", [128, DC, 512], BF16, es) for i in range(2)]
              stage = [sb("stage", [128, 512], F32, es) for i in range(2)]
              vstage = [sb("vstage", [128, 512], BF16, es) for i in range(2)]
              tstage = [sb("tstage", [128, 512], BF16, es) for i in range(2)]
              t1 = sb("t1", [128, 64], F32, es)
              for g in range(NG):
                  for s4 in range(4):
                      xb = xrow[cnt["xr"] % 2]
                      cnt["xr"] += 1
                      load_rows(x_src, r_src, g * 4 + s4, xb)
                      for c4 in range(0, DC, 4):
                          pb = P[cnt["p"] % 2]
                          cnt["p"] += 1
                          n = min(4, DC - c4)
                          for jj in range(n):
                              op("pe", "transpose", reads=[xb.r, identf.r], writes=[pb.r], out=pb.t[:, jj * 128:(jj + 1) * 128], in_=xb.t[:, (c4 + jj) * 128:(c4 + jj + 1) * 128], identity=identf.t[:])
                          src = pb.t[:, 0:n * 128].rearrange("p (c t) -> p c t", t=128)
                          if (c4 // 4) % 2 == 0:
                              op("act", "activation", reads=[pb.r], writes=[actT.r], out=actT.t[:, c4:c4 + n, s4 * 128:(s4 + 1) * 128], in_=src, func=AF.Copy)
                          else:
                              op("dve", "tensor_copy", reads=[pb.r], writes=[actT.r], out=actT.t[:, c4:c4 + n, s4 * 128:(s4 + 1) * 128], in_=src)
                  for cb in range(QKB):
                      wb = wblk[cnt["w"] % 2]
                      cnt["w"] += 1
                      dma("poolq", reads=[r_in], writes=[wb.r], out=wb.t[:], in_=wqkv[:, cb * 512:(cb + 1) * 512].rearrange("(c p) n -> p c n", p=128))
                      for s4 in range(4):
                          tt = g * 4 + s4
                          pa = P[2 + cnt["p"] % 2]
                          cnt["p"] += 1
                          for c in range(DC):
                              op("pe", "matmul", reads=[actT.r, wb.r], writes=[pa.r], out=pa.t[:], lhsT=actT.t[:, c, s4 * 128:(s4 + 1) * 128], rhs=wb.t[:, c, :], start=(c == 0), stop=(c == DC - 1))
                          st = stage[cnt["st"] % 2]
                          cnt["st"] += 1
                          op("act", "activation", reads=[pa.r], writes=[st.r], out=st.t[:], in_=pa.t[:], func=AF.Copy)
                          if cfg.get('noqk') and cb * 512 < 2 * D:
                              continue
                          if cb * 512 < 2 * D:
                              for m in range(0 if not cfg.get('norope') else 4, 4):
                                  a = st.t[:, m * 128:m * 128 + 16]
                                  b = st.t[:, m * 128 + 16:m * 128 + 32]
                                  op("dve", "tensor_tensor", reads=[st.r, cosT.r], writes=[t1.r], out=t1.t[:, 0:16], in0=a, in1=cos3[:, tt, :], op=ALU.mult)
                                  op("dve", "tensor_tensor", reads=[st.r, sinT.r], writes=[t1.r], out=t1.t[:, 16:32], in0=b, in1=sin3[:, tt, :], op=ALU.mult)
                                  op("dve", "tensor_tensor", reads=[st.r, cosT.r], writes=[t1.r], out=t1.t[:, 32:48], in0=b, in1=cos3[:, tt, :], op=ALU.mult)
                                  op("dve", "tensor_tensor", reads=[st.r, sinT.r], writes=[t1.r], out=t1.t[:, 48:64], in0=a, in1=sin3[:, tt, :], op=ALU.mult)
                                  op("dve", "tensor_tensor", reads=[t1.r, st.r], writes=[st.r], out=st.t[:, m * 128:m * 128 + 16], in0=t1.t[:, 0:16], in1=t1.t[:, 16:32], op=ALU.subtract)
                                  op("dve", "tensor_tensor", reads=[t1.r, st.r], writes=[st.r], out=st.t[:, m * 128 + 16:m * 128 + 32], in0=t1.t[:, 32:48], in1=t1.t[:, 48:64], op=ALU.add)
                              for m in range(4):
                                  op("pe", "transpose", reads=[st.r, identf.r], writes=[P[4].r], out=P[4].t[:, m * 128:(m + 1) * 128], in_=st.t[:, m * 128:(m + 1) * 128], identity=identf.t[:])
                              ts = tstage[cnt["ts"] % 2]
                              cnt["ts"] += 1
                              op("act", "activation", reads=[P[4].r], writes=[ts.r], out=ts.t[:], in_=P[4].t[:], func=AF.Copy)
                              isq = cb * 512 < D
                              dst, r_dst = (QT, r_QT) if isq else (KT, r_KT)
                              m0 = (cb * 512 - (0 if isq else D)) // 128
                              for mm in range(4):
                                  dma("sp", reads=[ts.r], writes=[r_dst], out=dst[m0 + mm, :, tt * 128:(tt + 1) * 128], in_=ts.t[:, mm * 128:(mm + 1) * 128])
                          else:
                              c0 = cb * 512 - 2 * D
                              vs = vstage[cnt["st"] % 2]
                              op("dve", "tensor_copy", reads=[st.r], writes=[vs.r], out=vs.t[:], in_=st.t[:])
                              dma("sp", reads=[vs.r], writes=[r_V], out=Vd[tt * 128:(tt + 1) * 128, c0:c0 + 512], in_=vs.t[:])
              Sc.barrier()
              if stop == 'A' + str(L):
                  raise _Stop()

          with ExitStack() as es:
              ktb = sb("ktb", [128, S], BF16, es)
              vb = sb("vb", [128, NT, 257], BF16, es)
              qtb = [sb("qtb", [128, 512], BF16, es) for i in range(2)]
              ssb = sb("ssb", [128, 512], F32, es)
              ptb = [sb("ptb", [128, 512], BF16, es) for i in range(3)]
              o1 = sb("o1", [128, NT, 256], F32, es)
              ot = sb("ot", [128, 256], F32, es)
              obf = sb("obf", [128, 256], F32, es)
              rc = sb("rc", [128, 1], F32, es)
              sq = sb("sq", [128, 256], F32, es)
              ssum = sb("ssum", [128, 1], F32, es)
              lamt = sb("lamt", [128, 4], F32, es)
              lv = [sb("lv", [128, 128], F32, es) for i in range(4)]
              gsub = sb("gsub", [128, 256], F32, es)
              kmean = sb("kmean", [128, NBP], F32, es)
              kmeanb = sb("kmeanb", [128, NBP], BF16, es)
              gate = sb("gate", [128, NBP], F32, es)
              selb = sb("selb", [128, 32], F32, es)
              selT = sb("selT", [32, 512], BF16, es)
              acnt = {"q": 0, "p": 0, "ps": 0}
              if is_diff:
                  for i, v in enumerate((lq1, lk1, lq2, lk2)):
                      dma("sp", reads=[r_in], writes=[lv[i].r], out=lv[i].t[:], in_=bc(v[j:j + 1, :]))
                  op("dve", "tensor_tensor", reads=[lv[0].r, lv[1].r], writes=[lv[0].r], out=lv[0].t[:], in0=lv[0].t[:], in1=lv[1].t[:], op=ALU.mult)
                  op("dve", "tensor_tensor", reads=[lv[2].r, lv[3].r], writes=[lv[2].r], out=lv[2].t[:], in0=lv[2].t[:], in1=lv[3].t[:], op=ALU.mult)
                  op("dve", "tensor_reduce", reads=[lv[0].r], writes=[lamt.r], out=lamt.t[:, 0:1], in_=lv[0].t[:], axis=AX.X, op=ALU.add)
                  op("dve", "tensor_reduce", reads=[lv[2].r, lamt.r], writes=[lamt.r], out=lamt.t[:, 1:2], in_=lv[2].t[:], axis=AX.X, op=ALU.add)
                  op("act", "activation", reads=[lamt.r], writes=[lamt.r], out=lamt.t[:, 0:2], in_=lamt.t[:, 0:2], func=AF.Exp)
                  op("dve", "tensor_tensor", reads=[lamt.r], writes=[lamt.r], out=lamt.t[:, 2:3], in0=lamt.t[:, 1:2], in1=lamt.t[:, 0:1], op=ALU.subtract)
                  op("dve", "tensor_scalar", reads=[lamt.r], writes=[lamt.r], out=lamt.t[:, 2:3], in0=lamt.t[:, 2:3], scalar1=-float(lambda_init), scalar2=None, op0=ALU.add)
                  dma("sp", reads=[r_in], writes=[gsub.r], out=gsub.t[:], in_=bc(subg[j:j + 1, :]))
                  op("dve", "tensor_scalar", reads=[gsub.r], writes=[gsub.r], out=gsub.t[:], in0=gsub.t[:], scalar1=float(1.0 - lambda_init), scalar2=None, op0=ALU.mult)
              for m in range(NM):
                  h = m // 2 if is_diff else m
                  which = m % 2 if is_diff else 0
                  dma("sp", reads=[r_KT], writes=[ktb.r], out=ktb.t[:], in_=KT[m, :, :])
                  if which == 0:
                      for t0 in range(0, NT, 16):
                        t1_ = min(NT, t0 + 16)
                        dma("sp", reads=[r_V], writes=[vb.r], out=vb.t[:, t0:t1_, 0:dv], in_=Vd[t0 * 128:t1_ * 128, h * dv:(h + 1) * dv].rearrange("(t p) v -> p t v", p=128))
                      op("dve", "memset", reads=[], writes=[vb.r], ap=vb.t[:, :, dv:dv + 1], constant=1.0)
                  if not is_diff:
                      op("dve", "memset", writes=[kmean.r], ap=kmean.t[:], constant=0.0)
                      op("dve", "tensor_reduce", reads=[ktb.r], writes=[kmean.r], out=kmean.t[:, 0:NB], in_=ktb.t[:].rearrange("d (n k) -> d n k", k=256), axis=AX.X, op=ALU.add)
                      op("dve", "tensor_scalar", reads=[kmean.r], writes=[kmeanb.r], out=kmeanb.t[:], in0=kmean.t[:], scalar1=1.0 / 256, scalar2=None, op0=ALU.mult)
                  for qg in range(NG):
                      qt = qtb[acnt["q"] % 2]
                      acnt["q"] += 1
                      dma("sp", reads=[r_QT], writes=[qt.r], out=qt.t[:], in_=QT[m, :, qg * 512:(qg + 1) * 512])
                      if not is_diff:
                          for s4 in range(4):
                              own = (qg * 512 + s4 * 128) // 256
                              op("dve", "memset", writes=[selb.r], ap=selb.t[:], constant=0.0)
                              if own > 3:
                                  pg = P[6]
                                  op("pe", "matmul", reads=[qt.r, kmeanb.r], writes=[pg.r], out=pg.t[:, 0:NBP], lhsT=qt.t[:, s4 * 128:(s4 + 1) * 128], rhs=kmeanb.t[:], start=True, stop=True)
                                  op("dve", "memset", writes=[gate.r], ap=gate.t[:], constant=-1e30)
                                  op("dve", "tensor_copy", reads=[pg.r], writes=[gate.r], out=gate.t[:, 0:own], in_=pg.t[:, 0:own])
                                  op("dve", "max", reads=[gate.r], writes=[m8.r], out=m8.t[:], in_=gate.t[:])
                                  op("dve", "tensor_scalar", reads=[gate.r, m8.r], writes=[selb.r], out=selb.t[:, 0:own], in0=gate.t[:, 0:own], scalar1=m8.t[:, 2:3], scalar2=-1.0, op0=ALU.is_ge, op1=ALU.add)
                                  op("dve", "tensor_scalar", reads=[selb.r], writes=[selb.r], out=selb.t[:, 0:own], in0=selb.t[:, 0:own], scalar1=-NEG, scalar2=None, op0=ALU.mult)
                              pt_ = P[5]
                              op("pe", "transpose", reads=[selb.r, identf.r], writes=[pt_.r], out=pt_.t[0:32, 0:128], in_=selb.t[:, 0:32], identity=identf.t[:])
                              op("act", "activation", reads=[pt_.r], writes=[selT.r], out=selT.t[:, s4 * 128:(s4 + 1) * 128], in_=pt_.t[0:32, 0:128], func=AF.Copy)
                      q0 = qg * 512
                      nkt = (q0 + 512) // 128
                      po = [P[i] for i in range(4)]
                      ps_bufs = (P[4], P[5]) if is_diff else (P[4], P[6])

                      def emit_qk(kt):
                          k0 = kt * 128
                          ps = ps_bufs[kt % 2]
                          use_sel = not is_diff
                          op("pe", "matmul", reads=[ktb.r, qt.r], writes=[ps.r], out=ps.t[:], lhsT=ktb.t[:, k0:k0 + 128], rhs=qt.t[:], start=True, stop=not use_sel)
                          if use_sel:
                              n = kt // 2
                              op("pe", "matmul", reads=[eoh.r, selT.r], writes=[ps.r], out=ps.t[:], lhsT=eoh.t[:, n * 128:(n + 1) * 128], rhs=selT.t[:], start=False, stop=True)

                      emit_qk(0)
                      for kt in range(nkt):
                          k0 = kt * 128
                          ps = ps_bufs[kt % 2]
                          if kt + 1 < nkt:
                              emit_qk(kt + 1)
                          pt = ptb[acnt["p"] % 3]
                          acnt["p"] += 1
                          if k0 >= q0:
                              cm = cmask[(k0 - q0) // 128]
                              op("dve", "tensor_tensor", reads=[ps.r, cm.r], writes=[ssb.r], out=ssb.t[:], in0=ps.t[:], in1=cm.t[:], op=ALU.add)
                              op("act", "activation", reads=[ssb.r], writes=[pt.r], out=pt.t[:], in_=ssb.t[:], func=AF.Exp, scale=scale)
                          else:
                              op("act", "activation", reads=[ps.r], writes=[pt.r], out=pt.t[:], in_=ps.t[:], func=AF.Exp, scale=scale)
                          for s4 in range(4):
                              qs0 = q0 + s4 * 128
                              if k0 > qs0 + 127:
                                  continue
                              last_kt = (qs0 + 127) // 128
                              op("pe", "matmul", reads=[pt.r, vb.r], writes=[po[s4].r], out=po[s4].t[:, 0:dv + 1], lhsT=pt.t[:, s4 * 128:(s4 + 1) * 128], rhs=vb.t[:, kt, 0:dv + 1], start=(kt == 0), stop=(kt == last_kt))
                      for s4 in range(4):
                          tt = qg * 4 + s4
                          pz = po[s4]
                          op("dve", "reciprocal", reads=[pz.r], writes=[rc.r], out=rc.t[:], in_=pz.t[:, dv:dv + 1])
                          if is_diff and which == 0:
                              op("dve", "tensor_scalar", reads=[pz.r, rc.r], writes=[o1.r], out=o1.t[:, tt, :], in0=pz.t[:, 0:dv], scalar1=rc.t[:, 0:1], scalar2=None, op0=ALU.mult)
                              continue
                          if is_diff:
                              op("dve", "tensor_scalar", reads=[pz.r, rc.r, lamt.r], writes=[ot.r], out=ot.t[:], in0=pz.t[:, 0:dv], scalar1=rc.t[:, 0:1], scalar2=lamt.t[:, 2:3], op0=ALU.mult, op1=ALU.mult)
                              op("dve", "tensor_tensor", reads=[ot.r, o1.r], writes=[ot.r], out=ot.t[:], in0=ot.t[:], in1=o1.t[:, tt, :], op=ALU.add)
                              op("act", "activation", reads=[ot.r], writes=[sq.r, ssum.r], out=sq.t[:], in_=ot.t[:], func=AF.Square, accum_out=ssum.t[:])
                              op("dve", "tensor_scalar", reads=[ssum.r], writes=[ssum.r], out=ssum.t[:], in0=ssum.t[:], scalar1=1.0 / 256, scalar2=float(cfg["rms_eps"]), op0=ALU.mult, op1=ALU.add)
                              op("act", "activation", reads=[ssum.r], writes=[ssum.r], out=ssum.t[:], in_=ssum.t[:], func=AF.Sqrt)
                              op("dve", "reciprocal", reads=[ssum.r], writes=[ssum.r], out=ssum.t[:], in_=ssum.t[:])
                              op("dve", "scalar_tensor_tensor", reads=[ot.r, ssum.r, gsub.r], writes=[obf.r], out=obf.t[:], in0=ot.t[:], scalar=ssum.t[:, 0:1], in1=gsub.t[:], op0=ALU.mult, op1=ALU.mult)
                          else:
                              op("dve", "tensor_scalar", reads=[pz.r, rc.r], writes=[obf.r], out=obf.t[:, 0:dv], in0=pz.t[:, 0:dv], scalar1=rc.t[:, 0:1], scalar2=None, op0=ALU.mult)
                          dma("sp", reads=[obf.r], writes=[r_AO], out=AO[tt * 128:(tt + 1) * 128, h * dv:(h + 1) * dv], in_=obf.t[:, 0:dv])
              Sc.barrier()
              if stop == 'B' + str(L):
                  raise _Stop()

          OB = max(1, D // 512)
          ncol = min(512, D)
          with ExitStack() as es:
              gbc = sb("gbc", [128, D], F32, es)
              bbc = sb("bbc", [128, D], F32, es)
              hrow = [sb("hrow", [128, D], F32, es) for i in range(2)]
              arow = [sb("arow", [128, D], F32, es) for i in range(2)]
              actT = sb("actT", [128, DC, 256], BF16, es)
              wblk = [sb("wblk", [128, DC, 512], BF16, es) for i in range(1)] * 2
              dma("sp", reads=[r_in], writes=[gbc.r], out=gbc.t[:], in_=bc(ln_mix_g[L:L + 1, :]))
              dma("sp", reads=[r_in], writes=[bbc.r], out=bbc.t[:], in_=bc(ln_mix_b[L:L + 1, :]))
              for g2 in range(S // 256):
                  for s2 in range(2):
                      ab = arow[s2]
                      load_rows(AO, r_AO, g2 * 2 + s2, ab)
                      for c4 in range(0, DC, 4):
                          n = min(4, DC - c4)
                          pb = P[4 + (c4 // 4) % 2]
                          for jj in range(n):
                              op("pe", "transpose", reads=[ab.r, identf.r], writes=[pb.r], out=pb.t[:, jj * 128:(jj + 1) * 128], in_=ab.t[:, (c4 + jj) * 128:(c4 + jj + 1) * 128], identity=identf.t[:])
                          op("act", "activation", reads=[pb.r], writes=[actT.r], out=actT.t[:, c4:c4 + n, s2 * 128:(s2 + 1) * 128], in_=pb.t[:, 0:n * 128].rearrange("p (c t) -> p c t", t=128), func=AF.Copy)
                      load_rows(x_src, r_src, g2 * 2 + s2, hrow[s2])
                  for cb in range(OB):
                      wb = wblk[cnt["w"] % 2]
                      cnt["w"] += 1
                      dma("poolq", reads=[r_in], writes=[wb.r], out=wb.t[:, :, 0:ncol], in_=wo[:, cb * 512:cb * 512 + ncol].rearrange("(c p) n -> p c n", p=128))
                      for s2 in range(2):
                          pa = P[2 + cnt["p"] % 2]
                          cnt["p"] += 1
                          for c in range(DC):
                              op("pe", "matmul", reads=[actT.r, wb.r], writes=[pa.r], out=pa.t[:, 0:ncol], lhsT=actT.t[:, c, s2 * 128:(s2 + 1) * 128], rhs=wb.t[:, c, 0:ncol], start=(c == 0), stop=(c == DC - 1))
                          hs = hrow[s2].t[:, cb * 512:cb * 512 + ncol]
                          op("dve", "scalar_tensor_tensor", reads=[hrow[s2].r, pa.r], writes=[hrow[s2].r], out=hs, in0=hs, scalar=alpha, in1=pa.t[:, 0:ncol], op0=ALU.mult, op1=ALU.add)
                  for s2 in range(2):
                      layer_norm_rows(hrow[s2], gbc, bbc, X1, r_X1, g2 * 2 + s2)
              Sc.barrier()
              if stop == 'C' + str(L):
                  raise _Stop()

          NBLK = (S * TOPK) // GB + E
          PR = NBLK * GB
          NH = GB // 128
          FCB = FC * 128
          out_ap, r_o = (y_out, r_y) if L == depth - 1 else (X2, r_X2)
          with ExitStack() as esD:
              W4all = sb("W4all", [128, NT, 4], F32, esD)
              D4all = sb("D4all", [128, NT, 4], I32, esD)
              ebf = sb("ebf", [128, NBLK], F32, esD)
              iot6 = sb("iot6", [128, FC], F32, esD)
              iotp = sb("iotp", [128, 1], F32, esD)
              ioti = sb("ioti", [128, FC], I32, esD)
              op("pool", "iota", writes=[ioti.r], out=ioti.t[:], pattern=[[128, FC]], base=0, channel_multiplier=1)
              op("dve", "tensor_copy", reads=[ioti.r], writes=[iot6.r], out=iot6.t[:], in_=ioti.t[:])
              op("dve", "tensor_copy", reads=[ioti.r], writes=[iotp.r], out=iotp.t[:], in_=ioti.t[:, 0:1])
              with ExitStack() as es:
                  wtmp = [sb("wtmp", [128, DC, 128], BF16, es) for i in range(2)]
                  wdt = [sb("wdt", [128, FC, D], BF16, es) for i in range(2)]
                  zrow = sb("zrow", [128, D], F32, es)
                  bgT = sb("bgT", [128, E * FC], F32, es)
                  brow = sb("brow", [E, F], F32, es)
                  op("dve", "memset", writes=[zrow.r], ap=zrow.t[:], constant=0.0)
                  for b in range(NBLK * NH):
                      for ch in range(NCH):
                          dma("sp", reads=[zrow.r], writes=[r_XS], out=XS[ch][b * 128:(b + 1) * 128, :], in_=zrow.t[:, ch * CW:(ch + 1) * CW])
                  k = 0
                  for e in range(E):
                      for (w_in_, WJ) in ((wg_in, WGJ), (wu_in, WUJ)):
                          for fc in range(FC):
                              wt = wtmp[k % 2]
                              k += 1
                              dma("poolq", reads=[r_in], writes=[wt.r], out=wt.t[:], in_=w_in_[L, e][:, fc * 128:(fc + 1) * 128].rearrange("(c p) f -> p c f", p=128))
                              c0_ = (fc % 2) * D
                              dma("sp", reads=[wt.r], writes=[r_W2], out=WJ[fc // 2][e * 128:(e + 1) * 128, c0_:c0_ + D], in_=wt.t[:].rearrange("p c f -> p (c f)"))
                      wd_ = wdt[e % 2]
                      dma("poolq", reads=[r_in], writes=[wd_.r], out=wd_.t[:], in_=wd_in[L, e].rearrange("(c p) n -> p c n", p=128))
                      dma("sp", reads=[wd_.r], writes=[r_W2], out=WD2[e * 128:(e + 1) * 128, :], in_=wd_.t[:].rearrange("p c n -> p (c n)"))
                  for bi_, b_in_ in enumerate((bg_in, bu_in)):
                      dma("sp", reads=[r_in], writes=[brow.r], out=brow.t[:], in_=b_in_[L])
                      bT3 = bgT.t[:].rearrange("p (e c) -> p e c", c=FC)
                      for fc in range(FC):
                          op("pe", "transpose", reads=[brow.r, identf.r], writes=[P[5].r], out=P[5].t[:, 0:E], in_=brow.t[:, fc * 128:(fc + 1) * 128], identity=identf.t[0:E, 0:E])
                          op("act", "activation", reads=[P[5].r], writes=[bgT.r], out=bT3[:, :, fc], in_=P[5].t[:, 0:E], func=AF.Copy)
                      dma("sp", reads=[bgT.r], writes=[r_W2], out=BGU2.rearrange("(e p) c -> p e c", p=128)[:, :, bi_ * FC:(bi_ + 1) * FC], in_=bT3, allow_slow_non_contiguous=True)
                  Sc.barrier()
              with ExitStack() as es:
                  Mall = sb("Mall", [128, NT, E], F32, es)
                  Wall = sb("Wall", [128, NT, E], F32, es)
                  Rall = sb("Rall", [128, NT, E], F32, es)
                  Bs = sb("Bs", [128, E], F32, es)
                  utri = sb("utri", [128, 128], BF16, es)
                  ones = sb("ones", [128, 128], BF16, es)
                  iou = sb("iou", [128, 128], I32, es)
                  xrow = [sb("xrow", [128, D], F32, es) for i in range(2)]
                  rw = sb("rw", [128, DC, E], F32, es)
                  rwh = sb("rwh", [128, DC, E], BF16, es)
                  rwl = sb("rwl", [128, DC, E], BF16, es)
                  rbb = sb("rbb", [128, E], F32, es)
                  xtf = [sb("xtf", [128, 128], F32, es) for i in range(2)]
                  xhi = [sb("xhi", [128, 128], BF16, es) for i in range(2)]
                  xlo = [sb("xlo", [128, 128], BF16, es) for i in range(2)]
                  logit = sb("logit", [128, E], F32, es)
                  mb = sb("mb", [128, E], BF16, es)
                  esum = sb("esum", [128, 1], F32, es)
                  cnti = sb("cnti", [128, E], I32, es)
                  padf = sb("padf", [128, E], F32, es)
                  pend = sb("pend", [128, E], F32, es)
                  pstart = sb("pstart", [128, E], F32, es)
                  tmpE = sb("tmpE", [128, E], F32, es)
                  key = sb("key", [128, E], F32, es)
                  d4f = sb("d4f", [128, 4], F32, es)
                  op("pool", "iota", writes=[iou.r], out=iou.t[:], pattern=[[1, 128]], base=0, channel_multiplier=-1)
                  op("dve", "tensor_scalar", reads=[iou.r], writes=[utri.r], out=utri.t[:], in0=iou.t[:], scalar1=0, scalar2=None, op0=ALU.is_gt)
                  op("dve", "memset", writes=[ones.r], ap=ones.t[:], constant=1.0)
                  op("dve", "memset", writes=[Bs.r], ap=Bs.t[:], constant=0.0)
                  dma("sp", reads=[r_in], writes=[rw.r], out=rw.t[:], in_=router_w[L].rearrange("(c p) e -> p c e", p=128))
                  op("dve", "tensor_copy", reads=[rw.r], writes=[rwh.r], out=rwh.t[:], in_=rw.t[:])
                  op("dve", "tensor_tensor", reads=[rw.r, rwh.r], writes=[rwl.r], out=rwl.t[:], in0=rw.t[:], in1=rwh.t[:], op=ALU.subtract)
                  dma("sp", reads=[r_in], writes=[rbb.r], out=rbb.t[:], in_=bc(router_b[L:L + 1, :]))
                  for tt in range(NT):
                      xb = xrow[tt % 2]
                      load_rows(X1, r_X1, tt, xb)
                      pl = P[6]
                      for c in range(DC):
                          pb = P[cnt["p"] % 2]
                          cnt["p"] += 1
                          op("pe", "transpose", reads=[xb.r, identf.r], writes=[pb.r], out=pb.t[:, 0:128], in_=xb.t[:, c * 128:(c + 1) * 128], identity=identf.t[:])
                          xf, xh, xl = xtf[c % 2], xhi[c % 2], xlo[c % 2]
                          op("act", "activation", reads=[pb.r], writes=[xf.r], out=xf.t[:], in_=pb.t[:, 0:128], func=AF.Copy)
                          op("dve", "tensor_copy", reads=[xf.r], writes=[xh.r], out=xh.t[:], in_=xf.t[:])
                          op("dve", "tensor_tensor", reads=[xf.r, xh.r], writes=[xl.r], out=xl.t[:], in0=xf.t[:], in1=xh.t[:], op=ALU.subtract)
                          op("pe", "matmul", reads=[xh.r, rwh.r], writes=[pl.r], out=pl.t[:, 0:E], lhsT=xh.t[:], rhs=rwh.t[:, c, :], start=(c == 0), stop=False)
                          op("pe", "matmul", reads=[xh.r, rwl.r], writes=[pl.r], out=pl.t[:, 0:E], lhsT=xh.t[:], rhs=rwl.t[:, c, :], start=False, stop=False)
                          op("pe", "matmul", reads=[xl.r, rwh.r], writes=[pl.r], out=pl.t[:, 0:E], lhsT=xl.t[:], rhs=rwh.t[:, c, :], start=False, stop=(c == DC - 1))
                      Mt = Mall.t[:, tt, :]
                      Wt = Wall.t[:, tt, :]
                      op("act", "activation", reads=[pl.r], writes=[logit.r], out=logit.t[:], in_=pl.t[:, 0:E], func=AF.Copy)
                      op("dve", "tensor_tensor", reads=[logit.r, rbb.r], writes=[logit.r], out=logit.t[:], in0=logit.t[:], in1=rbb.t[:], op=ALU.add)
                      op("dve", "max", reads=[logit.r], writes=[m8.r], out=m8.t[:], in_=logit.t[:])
                      op("dve", "tensor_scalar", reads=[logit.r, m8.r], writes=[Mall.r], out=Mt, in0=logit.t[:], scalar1=m8.t[:, TOPK - 1:TOPK], scalar2=None, op0=ALU.is_ge)
                      op("dve", "tensor_scalar", reads=[logit.r, m8.r], writes=[logit.r], out=logit.t[:], in0=logit.t[:], scalar1=m8.t[:, 0:1], scalar2=None, op0=ALU.subtract)
                      op("act", "activation", reads=[logit.r], writes=[logit.r], out=logit.t[:], in_=logit.t[:], func=AF.Exp)
                      op("dve", "tensor_tensor", reads=[logit.r, Mall.r], writes=[Wall.r], out=Wt, in0=logit.t[:], in1=Mt, op=ALU.mult)
                      op("dve", "tensor_reduce", reads=[Wall.r], writes=[esum.r], out=esum.t[:], in_=Wt, axis=AX.X, op=ALU.add)
                      op("dve", "reciprocal", reads=[esum.r], writes=[esum.r], out=esum.t[:], in_=esum.t[:])
                      op("dve", "tensor_scalar", reads=[Wall.r, esum.r], writes=[Wall.r], out=Wt, in0=Wt, scalar1=esum.t[:, 0:1], scalar2=None, op0=ALU.mult)
                      op("dve", "tensor_copy", reads=[Mall.r], writes=[mb.r], out=mb.t[:], in_=Mt)
                      pr, pc = P[2], P[3]
                      op("pe", "matmul", reads=[utri.r, mb.r], writes=[pr.r], out=pr.t[:, 0:E], lhsT=utri.t[:], rhs=mb.t[:], start=True, stop=True)
                      op("pe", "matmul", reads=[ones.r, mb.r], writes=[pc.r], out=pc.t[:, 0:E], lhsT=ones.t[:], rhs=mb.t[:], start=True, stop=True)
                      op("dve", "tensor_tensor", reads=[pr.r, Bs.r], writes=[Rall.r], out=Rall.t[:, tt, :], in0=pr.t[:, 0:E], in1=Bs.t[:], op=ALU.add)
                      op("dve", "tensor_tensor", reads=[pc.r, Bs.r], writes=[Bs.r], out=Bs.t[:], in0=pc.t[:, 0:E], in1=Bs.t[:], op=ALU.add)
                  op("dve", "tensor_scalar", reads=[Bs.r], writes=[cnti.r], out=cnti.t[:], in0=Bs.t[:], scalar1=float(GB - 1), scalar2=None, op0=ALU.add)
                  op("dve", "tensor_scalar", reads=[cnti.r], writes=[cnti.r], out=cnti.t[:], in0=cnti.t[:], scalar1=int(math.log2(GB)), scalar2=None, op0=ALU.arith_shift_right)
                  op("dve", "tensor_scalar", reads=[cnti.r], writes=[cnti.r], out=cnti.t[:], in0=cnti.t[:], scalar1=int(math.log2(GB)), scalar2=None, op0=ALU.logical_shift_left)
                  op("dve", "tensor_copy", reads=[cnti.r], writes=[padf.r], out=padf.t[:], in_=cnti.t[:])
                  op("dve", "tensor_copy", reads=[padf.r], writes=[pend.r], out=pend.t[:], in_=padf.t[:])
                  for e in range(1, E):
                      op("dve", "tensor_tensor", reads=[pend.r], writes=[pend.r], out=pend.t[:, e:e + 1], in0=pend.t[:, e:e + 1], in1=pend.t[:, e - 1:e], op=ALU.add)
                  op("dve", "tensor_tensor", reads=[pend.r, padf.r], writes=[pstart.r], out=pstart.t[:], in0=pend.t[:], in1=padf.t[:], op=ALU.subtract)
                  for b in range(NBLK):
                      op("dve", "tensor_scalar", reads=[pend.r], writes=[tmpE.r, ebf.r], out=tmpE.t[:], in0=pend.t[:], scalar1=float(GB * b), scalar2=0.0, op0=ALU.is_le, op1=ALU.add, accum_out=ebf.t[:, b:b + 1])
                  op("dve", "tensor_scalar", reads=[ebf.r], writes=[ebf.r], out=ebf.t[:], in0=ebf.t[:], scalar1=float(E - 1), scalar2=None, op0=ALU.min)
                  Sc.barrier()
                  for tt in range(NT):
                      xb = xrow[tt % 2]
                      load_rows(X1, r_X1, tt, xb)
                      Mt = Mall.t[:, tt, :]
                      Wt = Wall.t[:, tt, :]
                      op("dve", "tensor_tensor", reads=[Rall.r, pstart.r], writes=[key.r], out=key.t[:], in0=Rall.t[:, tt, :], in1=pstart.t[:], op=ALU.add)
                      op("dve", "scalar_tensor_tensor", reads=[key.r, Mall.r], writes=[key.r], out=key.t[:], in0=key.t[:], scalar=1.0, in1=Mt, op0=ALU.add, op1=ALU.mult)
                      op("dve", "max", reads=[key.r], writes=[m8.r], out=m8.t[:], in_=key.t[:])
                      op("dve", "tensor_scalar", reads=[m8.r], writes=[d4f.r], out=d4f.t[:], in0=m8.t[:, 0:4], scalar1=-1.0, scalar2=None, op0=ALU.add)
                      op("dve", "tensor_copy", reads=[d4f.r], writes=[D4all.r], out=D4all.t[:, tt, :], in_=d4f.t[:])
                      for k in range(4):
                          op("dve", "tensor_scalar", reads=[key.r, m8.r], writes=[tmpE.r], out=tmpE.t[:], in0=key.t[:], scalar1=m8.t[:, k:k + 1], scalar2=None, op0=ALU.is_equal)
                          op("dve", "tensor_tensor", reads=[tmpE.r, Wall.r], writes=[tmpE.r], out=tmpE.t[:], in0=tmpE.t[:], in1=Wt, op=ALU.mult)
                          op("dve", "tensor_reduce", reads=[tmpE.r], writes=[W4all.r], out=W4all.t[:, tt, k:k + 1], in_=tmpE.t[:], axis=AX.X, op=ALU.add)
                      for k in range(4):
                          for ch in range(NCH):
                              Sc.dma_fn("poolq", "indirect_dma_start", reads=[xb.r, D4all.r], writes=[r_XS], out=XS[ch][:, :], out_offset=bass.IndirectOffsetOnAxis(ap=D4all.t[:, tt, k:k + 1], axis=0), in_=xb.t[:, ch * CW:(ch + 1) * CW], in_offset=None)
                  Sc.barrier()
              with ExitStack() as es:
                  xrow = [sb("xrow", [128, D], F32, es)] * 2
                  xT = sb("xT", [128, DC, GB], BF16, es)
                  wgb = [sb("wgb", [128, 2 * D], BF16, es) for i in range(2)]
                  wub = [sb("wub", [128, 2 * D], BF16, es) for i in range(2)]
                  wdb = sb("wdb", [128, FC * D], BF16, es)
                  yrow = [sb("yrow", [128, 512], F32, es) for i in range(2)]
                  bdrow = sb("bdrow", [128, D], F32, es)
                  bgub = sb("bgub", [128, 2 * FC], F32, es)
                  idx6 = [sb("idx6", [128, FC], I32, es) for i in range(2)]
                  idxb = [sb("idxb", [128, 1], I32, es) for i in range(2)]
                  idxe = [sb("idxe", [128, 1], I32, es) for i in range(2)]
                  gt = sb("gt", [128, GB], F32, es)
                  sg = sb("sg", [128, GB], F32, es)
                  ut = sb("ut", [128, GB], F32, es)
                  hT = sb("hT", [128, FC, GB], BF16, es)
                  for b in range(NBLK):
                      ib, ie = idxb[b % 2], idxe[b % 2]
                      eb = ebf.t[:, b:b + 1]
                      op("dve", "tensor_scalar", reads=[ebf.r, iotp.r], writes=[ib.r], out=ib.t[:], in0=eb, scalar1=128.0, scalar2=iotp.t[:, 0:1], op0=ALU.mult, op1=ALU.add)
                      op("dve", "tensor_scalar", reads=[ebf.r], writes=[ie.r], out=ie.t[:], in0=eb, scalar1=float(L * E), scalar2=None, op0=ALU.add)
                      for hf in range(NH):
                          xb = xrow[hf % 2]
                          r0_ = b * GB + hf * 128
                          for ch in range(NCH):
                              dma("sp", reads=[r_XS], writes=[xb.r], out=xb.t[:, ch * CW:(ch + 1) * CW], in_=XS[ch][r0_:r0_ + 128, :])
                          for c4 in range(0, DC, 4):
                              pb = P[cnt["p"] % 2]
                              cnt["p"] += 1
                              n = min(4, DC - c4)
                              for jj in range(n):
                                  op("pe", "transpose", reads=[xb.r, identf.r], writes=[pb.r], out=pb.t[:, jj * 128:(jj + 1) * 128], in_=xb.t[:, (c4 + jj) * 128:(c4 + jj + 1) * 128], identity=identf.t[:])
                              src = pb.t[:, 0:n * 128].rearrange("p (c t) -> p c t", t=128)
                              if (c4 // 4) % 2 == 0:
                                  op("act", "activation", reads=[pb.r], writes=[xT.r], out=xT.t[:, c4:c4 + n, hf * 128:(hf + 1) * 128], in_=src, func=AF.Copy)
                              else:
                                  op("dve", "tensor_copy", reads=[pb.r], writes=[xT.r], out=xT.t[:, c4:c4 + n, hf * 128:(hf + 1) * 128], in_=src)
                      Sc.dma_fn("poolq", "indirect_dma_start", reads=[r_W2, ib.r], writes=[bgub.r], out=bgub.t[:], out_offset=None, in_=BGU2[:, :], in_offset=bass.IndirectOffsetOnAxis(ap=ib.t[:, 0:1], axis=0))
                      Sc.dma_fn("poolq", "indirect_dma_start", reads=[r_in, ie.r], writes=[bdrow.r], out=bdrow.t[:], out_offset=None, in_=bd_in.rearrange("l e d -> (l e) d"), in_offset=bass.IndirectOffsetOnAxis(ap=ie.t[:, 0:1], axis=0))
                      for fc in range(FC):
                          jg, hh = fc // 2, fc % 2
                          wg_, wu_ = wgb[jg % 2], wub[jg % 2]
                          if hh == 0:
                              Sc.dma_fn("poolq", "indirect_dma_start", reads=[r_W2, ib.r], writes=[wg_.r], out=wg_.t[:], out_offset=None, in_=WGJ[jg][:, :], in_offset=bass.IndirectOffsetOnAxis(ap=ib.t[:, 0:1], axis=0))
                              Sc.dma_fn("poolq", "indirect_dma_start", reads=[r_W2, ib.r], writes=[wu_.r], out=wu_.t[:], out_offset=None, in_=WUJ[jg][:, :], in_offset=bass.IndirectOffsetOnAxis(ap=ib.t[:, 0:1], axis=0))
                          if fc == min(1, FC - 1):
                              Sc.dma_fn("poolq", "indirect_dma_start", reads=[r_W2, ib.r], writes=[wdb.r], out=wdb.t[:], out_offset=None, in_=WD2[:, :], in_offset=bass.IndirectOffsetOnAxis(ap=ib.t[:, 0:1], axis=0))
                          wo_ = hh * D
                          pgt, put = P[2], P[3]
                          for c in range(DC):
                              op("pe", "matmul", reads=[wg_.r, xT.r], writes=[pgt.r], out=pgt.t[:, 0:GB], lhsT=wg_.t[:, wo_ + c * 128:wo_ + (c + 1) * 128], rhs=xT.t[:, c, :], start=(c == 0), stop=(c == DC - 1))
                          for c in range(DC):
                              op("pe", "matmul", reads=[wu_.r, xT.r], writes=[put.r], out=put.t[:, 0:GB], lhsT=wu_.t[:, wo_ + c * 128:wo_ + (c + 1) * 128], rhs=xT.t[:, c, :], start=(c == 0), stop=(c == DC - 1))
                          op("dve", "tensor_scalar", reads=[pgt.r, bgub.r], writes=[gt.r], out=gt.t[:], in0=pgt.t[:, 0:GB], scalar1=bgub.t[:, fc:fc + 1], scalar2=float(cfg["limit"]), op0=ALU.add, op1=ALU.min)
                          op("act", "activation", reads=[gt.r], writes=[sg.r], out=sg.t[:], in_=gt.t[:], func=AF.Sigmoid, scale=float(cfg["swiglu_alpha"]))
                          op("dve", "tensor_scalar", reads=[put.r, bgub.r], writes=[ut.r], out=ut.t[:], in0=put.t[:, 0:GB], scalar1=bgub.t[:, FC + fc:FC + fc + 1], scalar2=float(cfg["limit"]), op0=ALU.add, op1=ALU.min)
                          op("dve", "tensor_scalar", reads=[ut.r], writes=[ut.r], out=ut.t[:], in0=ut.t[:], scalar1=-float(cfg["limit"]), scalar2=1.0, op0=ALU.max, op1=ALU.add)
                          op("dve", "tensor_tensor", reads=[gt.r, sg.r], writes=[gt.r], out=gt.t[:], in0=gt.t[:], in1=sg.t[:], op=ALU.mult)
                          op("dve", "tensor_tensor", reads=[gt.r, ut.r], writes=[hT.r], out=hT.t[:, fc, :], in0=gt.t[:], in1=ut.t[:], op=ALU.mult)
                      for hf in range(NH):
                        r0_ = b * GB + hf * 128
                        for cb in range(OB):
                          pa = P[4 + (hf * OB + cb) % 2]
                          for fc in range(FC):
                              op("pe", "matmul", reads=[hT.r, wdb.r], writes=[pa.r], out=pa.t[:, 0:ncol], lhsT=hT.t[:, fc, hf * 128:(hf + 1) * 128], rhs=wdb.t[:, fc * D + cb * 512:fc * D + cb * 512 + ncol], start=(fc == 0), stop=(fc == FC - 1))
                          yr = yrow[(hf * OB + cb) % 2]
                          op("dve", "tensor_tensor", reads=[pa.r, bdrow.r], writes=[yr.r], out=yr.t[:, 0:ncol], in0=pa.t[:, 0:ncol], in1=bdrow.t[:, cb * 512:cb * 512 + ncol], op=ALU.add)
                          ch, co = (cb * 512) // CW, (cb * 512) % CW
                          dma("sp", reads=[yr.r], writes=[r_YS], out=YS[ch][r0_:r0_ + 128, co:co + ncol], in_=yr.t[:, 0:ncol])
                  Sc.barrier()
              with ExitStack() as es:
                  gbc = sb("gbc", [128, D], F32, es)
                  bbc = sb("bbc", [128, D], F32, es)
                  hrow = [sb("hrow", [128, D], F32, es) for i in range(2)]
                  grow = [sb("grow", [128, D], F32, es) for i in range(4)]
                  dma("sp", reads=[r_in], writes=[gbc.r], out=gbc.t[:], in_=bc(ln_ffn_g[L:L + 1, :]))
                  dma("sp", reads=[r_in], writes=[bbc.r], out=bbc.t[:], in_=bc(ln_ffn_b[L:L + 1, :]))
                  for tt in range(NT):
                      hb = hrow[tt % 2]
                      load_rows(X1, r_X1, tt, hb)
                      for k in range(4):
                          for ch in range(NCH):
                              Sc.dma_fn("poolq", "indirect_dma_start", reads=[r_YS, D4all.r], writes=[grow[k].r], out=grow[k].t[:, ch * CW:(ch + 1) * CW], out_offset=None, in_=YS[ch][:, :], in_offset=bass.IndirectOffsetOnAxis(ap=D4all.t[:, tt, k:k + 1], axis=0))
                      op("dve", "tensor_scalar", reads=[hb.r], writes=[hb.r], out=hb.t[:], in0=hb.t[:], scalar1=alpha, scalar2=None, op0=ALU.mult)
                      for k in range(4):
                          op("dve", "scalar_tensor_tensor", reads=[grow[k].r, W4all.r, hb.r], writes=[hb.r], out=hb.t[:], in0=grow[k].t[:], scalar=W4all.t[:, tt, k:k + 1], in1=hb.t[:], op0=ALU.mult, op1=ALU.add)
                      layer_norm_rows(hb, gbc, bbc, out_ap, r_o, tt)
                  Sc.barrier()
              if stop == 'D' + str(L):
                  raise _Stop()


    except _Stop:
        dsrc = {'X1': (X1, r_X1), 'X2': (X2, r_X2), 'AO': (AO, r_AO)}.get(cfg.get('dump'))
        if dsrc is not None:
            for r0 in range(0, S, 512):
                dma("sp", reads=[dsrc[1]], writes=[r_y], out=y_out[r0:r0 + 512, :], in_=dsrc[0][r0:r0 + 512, :])
    Sc.wait_all("sp", [r_y.w] if r_y.w else [])
    fin = [(ent[0], ent[1]) for q in Sc.dma_sems for ent in Sc.dma_sems[q] if ent[1] > 0]
    Sc.wait_all("sp", fin)
    Sc.emit()
    return nc


FULL_CFG = dict(D=4096, S=8192, E=32, F=768, depth=2, topk=4, theta=500000.0,
                ln_eps=1e-5, rms_eps=1e-5, limit=7.0, swiglu_alpha=1.702)

_NAMES = ["diff_w_qkv", "diff_lambda_q1", "diff_lambda_k1", "diff_lambda_q2", "diff_lambda_k2",
          "diff_subln_g", "diff_w_o", "moba_w_qkv", "moba_w_o", "ln_mix_g", "ln_mix_b", "router_w",
          "router_b", "exp_w_gate", "exp_b_gate", "exp_w_up", "exp_b_up", "exp_w_down", "exp_b_down",
          "ln_ffn_g", "ln_ffn_b"]


def run_cfg(cfg, inputs):
    nc = build_program(cfg)
    B = inputs["x"].shape[0]
    in_maps = []
    for b in range(B):
        m = {"x": np.ascontiguousarray(inputs["x"][b], dtype=np.float32),
             "positions": np.ascontiguousarray(np.asarray(inputs["positions"][b]).reshape(-1, 1), dtype=np.int32)}
        for n in _NAMES:
            m[n] = np.ascontiguousarray(inputs[n], dtype=np.float32)
        in_maps.append(m)
    res = run_bass_kernel_spmd(nc, in_maps, core_ids=list(range(B)))
    return np.stack([np.asarray(r["y"]) for r in res.results], axis=0).astype(np.float32)


def kernel(**inputs):
    inputs = {k: np.asarray(v) for k, v in inputs.items()}
    return run_cfg(FULL_CFG, inputs)
```
